# Optimizing a Trainium2 kernel written in Bass

```python
import jax, jax.numpy as jnp
from jax import lax
import numpy as np

D_MODEL = 2048
BATCH = 4
SEQ = 4096
DEPTH = 1

HEAD_DIM = 128
N_Q_HEADS = 8
N_KV_HEADS = 2
Q_GROUP = N_Q_HEADS // N_KV_HEADS
ATTN_W = N_Q_HEADS * HEAD_DIM
KV_W = N_KV_HEADS * HEAD_DIM
Q_BLOCK = 128
ROPE_AXIS_DIM = HEAD_DIM // 2
ROPE_THETA = 10000.0
GRID_W = 64
CONV_W = D_MODEL // 2
CONV_K = 3
OFF_Q = 0
OFF_K = OFF_Q + ATTN_W
OFF_V = OFF_K + KV_W
OFF_CH = OFF_V + KV_W
OFF_CB = OFF_CH + CONV_W
OFF_CC = OFF_CB + CONV_W
OFF_GA = OFF_CC + CONV_W
OFF_GC = OFF_GA + D_MODEL
W_IN = OFF_GC + D_MODEL
PEER_HEADS = 8
PEER_N_KEYS = 128
PEER_N_EXPERTS = PEER_N_KEYS * PEER_N_KEYS
PEER_DK = 256
PEER_HALF = PEER_DK // 2
PEER_TOPK = 16
PEER_CHUNK = 128
PLE_DIM = 256
EPS = 1e-6

kernel_name = 'hybrid_gqa_shortconv_peer_encoder_block'


def rms_norm(x, g):
    xf = x.astype(jnp.float32)
    y = xf * lax.rsqrt(jnp.mean(xf * xf, axis=-1, keepdims=True) + EPS)
    return (y * g.astype(jnp.float32)).astype(x.dtype)


def axial_rope_tables(S):
    rows = S // GRID_W
    row = jnp.repeat(jnp.arange(rows, dtype=jnp.int32), GRID_W, total_repeat_length=S)
    col = jnp.tile(jnp.arange(GRID_W, dtype=jnp.int32), rows)
    inv = ROPE_THETA ** (-jnp.arange(0, ROPE_AXIS_DIM, 2, dtype=jnp.float32) / ROPE_AXIS_DIM)
    ang_r = row.astype(jnp.float32)[:, None] * inv[None, :]
    ang_c = col.astype(jnp.float32)[:, None] * inv[None, :]
    ang = jnp.concatenate([ang_r, ang_r, ang_c, ang_c], axis=-1)
    return jnp.cos(ang), jnp.sin(ang)


def apply_rope(x, cos, sin):
    xf = x.astype(jnp.float32)
    xs = xf.reshape(*x.shape[:-1], 2, 2, ROPE_AXIS_DIM // 2)
    rot = jnp.stack([-xs[..., 1, :], xs[..., 0, :]], axis=-2).reshape(x.shape)
    return (xf * cos[None, :, None, :] + rot * sin[None, :, None, :]).astype(x.dtype)


def block_attention(q, k, v):
    B, S = q.shape[0], q.shape[1]
    nblk = S // Q_BLOCK
    qb = q.reshape(B, nblk, Q_BLOCK, N_KV_HEADS, Q_GROUP, HEAD_DIM).transpose(1, 0, 3, 4, 2, 5)
    kt = k.transpose(0, 2, 1, 3)
    vt = v.transpose(0, 2, 1, 3)
    scale = HEAD_DIM ** -0.5

    def one_block(qblk):
        s = jnp.einsum('bkgqd,bksd->bkgqs', qblk, kt).astype(jnp.float32) * scale
        pr = jax.nn.softmax(s, axis=-1).astype(vt.dtype)
        return jnp.einsum('bkgqs,bksd->bkgqd', pr, vt)

    o = lax.map(one_block, qb)
    return o.transpose(1, 0, 4, 2, 3, 5).reshape(B, S, ATTN_W)


def short_conv(u, w):
    return lax.conv_general_dilated(
        u, w[:, None, :].astype(u.dtype), window_strides=(1,), padding=((1, 1),),
        dimension_numbers=('NWC', 'WIO', 'NWC'), feature_group_count=u.shape[-1])


def peer(h, w_q, sub_keys, u, v):
    B, S, D = h.shape
    T = B * S
    hc = h.reshape(T // PEER_CHUNK, PEER_CHUNK, D)

    def one_chunk(xc):
        q = (xc @ w_q).reshape(PEER_CHUNK, PEER_HEADS, 2, PEER_HALF)
        s = jnp.einsum('thpc,hpnc->thpn', q, sub_keys).astype(jnp.float32)
        sv, si = lax.top_k(s, PEER_TOPK)
        cand = (sv[:, :, 0, :, None] + sv[:, :, 1, None, :]).reshape(PEER_CHUNK, PEER_HEADS, PEER_TOPK * PEER_TOPK)
        cidx = (si[:, :, 0, :, None] * PEER_N_KEYS + si[:, :, 1, None, :]).reshape(PEER_CHUNK, PEER_HEADS, PEER_TOPK * PEER_TOPK)
        tv, tpos = lax.top_k(cand, PEER_TOPK)
        eidx = jnp.take_along_axis(cidx, tpos, axis=-1)
        gate = jax.nn.softmax(tv, axis=-1).astype(xc.dtype)
        a = jnp.einsum('td,thkd->thk', xc, u[eidx])
        hid = jax.nn.gelu(a, approximate=False) * gate
        return jnp.einsum('thk,thkd->td', hid, v[eidx])

    return lax.map(one_chunk, hc).reshape(B, S, D)


def setup_inputs(seed: int = 0) -> dict:
    key = jax.random.key(seed)
    ks = jax.random.split(key, 20)
    f32 = jnp.float32
    nrm = lambda k, shape, s: jax.random.normal(k, shape, f32) * s
    gain = lambda k, shape: 1.0 + 0.05 * jax.random.normal(k, shape, f32)
    return {
        'x': nrm(ks[0], (BATCH, SEQ, D_MODEL), 1.0),
        'p': nrm(ks[1], (DEPTH, BATCH, SEQ, PLE_DIM), 1.0),
        'g_mix': gain(ks[2], (DEPTH, D_MODEL)),
        'w_in': nrm(ks[3], (DEPTH, D_MODEL, W_IN), D_MODEL ** -0.5),
        'g_q': gain(ks[4], (DEPTH, HEAD_DIM)),
        'g_k': gain(ks[5], (DEPTH, HEAD_DIM)),
        'conv_w': nrm(ks[6], (DEPTH, CONV_K, CONV_W), CONV_K ** -0.5),
        'w_attn_out': nrm(ks[7], (DEPTH, ATTN_W, D_MODEL), ATTN_W ** -0.5),
        'w_conv_out': nrm(ks[8], (DEPTH, CONV_W, D_MODEL), CONV_W ** -0.5),
        'w_out': nrm(ks[9], (DEPTH, D_MODEL, D_MODEL), D_MODEL ** -0.5),
        'g_ffn': gain(ks[10], (DEPTH, D_MODEL)),
        'w_peer_q': nrm(ks[11], (DEPTH, D_MODEL, PEER_HEADS * PEER_DK), D_MODEL ** -0.5),
        'peer_sub_keys': nrm(ks[12], (DEPTH, PEER_HEADS, 2, PEER_N_KEYS, PEER_HALF), PEER_HALF ** -0.5),
        'peer_u': nrm(ks[13], (DEPTH, PEER_N_EXPERTS, D_MODEL), D_MODEL ** -0.5),
        'peer_v': nrm(ks[14], (DEPTH, PEER_N_EXPERTS, D_MODEL), PEER_HEADS ** -0.5),
        'g_ple': gain(ks[15], (DEPTH, D_MODEL)),
        'w_ple': nrm(ks[16], (DEPTH, PLE_DIM, D_MODEL), PLE_DIM ** -0.5),
        'w_ple_gate': nrm(ks[17], (DEPTH, D_MODEL, D_MODEL), D_MODEL ** -0.5),
    }


def reference(x, p, g_mix, w_in, g_q, g_k, conv_w, w_attn_out, w_conv_out, w_out,
              g_ffn, w_peer_q, peer_sub_keys, peer_u, peer_v, g_ple, w_ple, w_ple_gate):
    B, S, _ = x.shape
    cos, sin = axial_rope_tables(S)
    for i in range(DEPTH):
        h = rms_norm(x, g_mix[i])
        z = h @ w_in[i]
        q = z[..., OFF_Q:OFF_K].reshape(B, S, N_Q_HEADS, HEAD_DIM)
        k = z[..., OFF_K:OFF_V].reshape(B, S, N_KV_HEADS, HEAD_DIM)
        v = z[..., OFF_V:OFF_CH].reshape(B, S, N_KV_HEADS, HEAD_DIM)
        c_h = z[..., OFF_CH:OFF_CB]
        c_b = z[..., OFF_CB:OFF_CC]
        c_c = z[..., OFF_CC:OFF_GA]
        gate_a = z[..., OFF_GA:OFF_GC]
        gate_c = z[..., OFF_GC:W_IN]
        q = apply_rope(rms_norm(q, g_q[i]), cos, sin)
        k = apply_rope(rms_norm(k, g_k[i]), cos, sin)
        y_attn = block_attention(q, k, v) @ w_attn_out[i]
        y_conv = (c_b * short_conv(c_c * c_h, conv_w[i])) @ w_conv_out[i]
        merged = jax.nn.sigmoid(gate_a) * y_attn + jax.nn.sigmoid(gate_c) * y_conv
        x = x + merged @ w_out[i]
        x = x + peer(rms_norm(x, g_ffn[i]), w_peer_q[i], peer_sub_keys[i], peer_u[i], peer_v[i])
        x = x + (p[i] @ w_ple[i]) * jax.nn.sigmoid(rms_norm(x, g_ple[i]) @ w_ple_gate[i])
    return x
```

```python
import os
import numpy as np
from contextlib import ExitStack
import concourse.bass as bass
import concourse.mybir as mybir
from concourse.bass_utils import run_bass_kernel_spmd

F32 = mybir.dt.float32
BF16 = mybir.dt.bfloat16
U32 = mybir.dt.uint32
I32 = mybir.dt.int32
AF = mybir.ActivationFunctionType
ALU = mybir.AluOpType
AX = mybir.AxisListType

ENGS = ["pe", "act", "dve", "pool", "sp"]
NOSELF = set(os.environ.get("K_NOSELF", "").split(",")) - {""}


class Tile:
    def __init__(self, prog, handle, name):
        self.prog = prog
        self.h = handle
        self.name = name
        self.wev = None
        self.revs = []
        self.dsem = None
        self.dcnt = 0

    def __getitem__(self, idx):
        return self.h[idx]


class Prog:
    def __init__(self, nc, es):
        self.nc = nc
        self.es = es
        self.q = {e: [] for e in ENGS}
        self.cnt = {e: 0 for e in ENGS}
        self.sem = {}
        for e in ["pe", "act", "dve", "pool"]:
            self.sem[e] = es.enter_context(nc.semaphore("s_" + e))
        self.seen = {e: {} for e in ENGS}
        self.n_ops = 0

    def sbuf(self, name, shape, dt):
        h = self.es.enter_context(self.nc.sbuf_tensor(name, list(shape), dt))
        return Tile(self, h, name)

    def psum(self, name, shape, dt):
        h = self.es.enter_context(self.nc.psum_tensor(name, list(shape), dt))
        return Tile(self, h, name)

    def _dsem(self, t):
        if t.dsem is None:
            t.dsem = self.es.enter_context(self.nc.semaphore("d_" + t.name))
        return t.dsem

    def _collect(self, eng, reads, writes, is_dma):
        waits = []
        for t in reads:
            if t.wev is not None:
                waits.append(t.wev)
        for t in writes:
            if t.wev is not None:
                if not (is_dma and t.wev[3]):
                    waits.append(t.wev)
            waits.extend(t.revs)
        out = []
        seen = self.seen[eng]
        for (sem, val, weng, wdma) in waits:
            if eng == "pe" and weng == "pe" and not wdma:
                continue
            if weng == eng and not wdma and eng in NOSELF:
                continue
            key = id(sem)
            if seen.get(key, 0) >= val:
                continue
            seen[key] = val
            out.append((sem, val))
        return out

    def op(self, eng, fn, reads=(), writes=(), nowait=()):
        reads = [t for t in reads if t is not None]
        writes = [t for t in writes if t is not None]
        waits = self._collect(eng, reads, writes, False)
        writes = writes + list(nowait)
        self.cnt[eng] += 1
        ev = (self.sem[eng], self.cnt[eng], eng, False)
        self.q[eng].append((waits, fn, (self.sem[eng], 1)))
        for t in writes:
            t.wev = ev
            t.revs = []
        for t in reads:
            if t not in writes:
                t.revs = [r for r in t.revs if r[0] is not ev[0]] + [ev]
        self.n_ops += 1

    def dma(self, eng, fn, tile, is_write, extra_reads=()):
        reads = list(extra_reads) + ([] if is_write else [tile])
        writes = [tile] if is_write else []
        waits = self._collect(eng, reads, writes, True)
        sem = self._dsem(tile)
        tile.dcnt += 16
        ev = (sem, tile.dcnt, eng, True)
        self.q[eng].append((waits, fn, (sem, 16)))
        if is_write:
            tile.wev = ev
            tile.revs = []
        else:
            tile.revs = [r for r in tile.revs if r[0] is not sem] + [ev]
        for t in extra_reads:
            t.revs = [r for r in t.revs if r[0] is not sem] + [ev]
        self.n_ops += 1

    def final_wait(self, eng, tiles):
        waits = []
        for t in tiles:
            waits.extend(t.revs)
            if t.wev is not None:
                waits.append(t.wev)
        best = {}
        for (sem, val, _e, _d) in waits:
            k = id(sem)
            if k not in best or best[k][1] < val:
                best[k] = (sem, val)
        self.q[eng].append((list(best.values()), None, None))

    def emit(self):
        nc = self.nc

        def replay(ename, e):
            for (waits, fn, inc) in self.q[ename]:
                for (sem, val) in waits:
                    e.wait_ge(sem, val)
                if fn is not None:
                    ins = fn(e)
                    ins.then_inc(inc[0], inc[1])

        with nc.Block() as block:
            @block.tensor
            def _(e):
                replay("pe", e)

            @block.scalar
            def _(e):
                replay("act", e)

            @block.vector
            def _(e):
                replay("dve", e)

            @block.gpsimd
            def _(e):
                replay("pool", e)

            @block.sync
            def _(e):
                replay("sp", e)


D = 2048
DC = 16
SEQ = 4096
OWN = 2048
T = 256
NT = OWN // T
NTA = SEQ // T
HD = 128
NEXP = 16384
EPS = 1e-6
CQ, CK, CCH, CCB, CCC, CGA, CGC = 0, 8, 12, 20, 28, 36, 52
VG_MIX, VG_FFN, VG_PLE, VG_Q, VG_K, VCW = 0, 16, 32, 48, 49, 50
NVEC = 80


def build_program(nt_run=NT, do_peer=True, dbg=None):
    nc = bass.Bass("TRN2", target_bir_lowering=False)

    def din(name, shape, dt=F32):
        return nc.dram_tensor(name, list(shape), dt, kind="ExternalInput").ap()

    xT = din("xT", [D, SEQ])
    xhalo = din("xhalo", [128, NT, DC, 2])
    pT = din("pT", [256, OWN])
    cs = din("cs", [128, 2, SEQ])
    vecs = din("vecs", [128, NVEC])
    gqk = din("gqk", [2, 128])
    consts = din("consts", [128, 2, 128])
    win = din("win", [68, 128, DC, 128])
    wv = din("wv", [128, DC, 256])
    wao = din("wao", [16, 128, 8, 128])
    wco = din("wco", [16, 128, 8, 128])
    wo = din("wo", [16, 128, DC, 128])
    wpq = din("wpq", [16, 128, DC, 128])
    wpg = din("wpg", [16, 128, DC, 128])
    wpl = din("wpl", [16, 128, 2, 128])
    keysT = din("keysT", [128, 16, 128])
    pu = din("pu", [NEXP, D])
    pv = din("pv", [NEXP, D])
    outT = nc.dram_tensor("outT", [D, OWN], F32, kind="ExternalOutput").ap()
    pu_bf = nc.dram_tensor("pu_bf", [NEXP, D], BF16, kind="Internal").ap()
    pv_bf = nc.dram_tensor("pv_bf", [NEXP, D], BF16, kind="Internal").ap()
    wsrc = {"win": (win, 68, DC), "wao": (wao, 16, 8), "wco": (wco, 16, 8), "wo": (wo, 16, DC),
            "wpq": (wpq, 16, DC), "wpg": (wpg, 16, DC), "wpl": (wpl, 16, 2)}
    wbf = {k: nc.dram_tensor(k + "_bf", [n, 128, kc, 128], BF16, kind="Internal").ap() for k, (a, n, kc) in wsrc.items()}
    dbg_outs = {}

    xT_v = xT.rearrange("(dc p) t -> p dc t", p=128)
    outT_v = outT.rearrange("(dc p) t -> p dc t", p=128)
    pT_v = pT.rearrange("(kc p) t -> p kc t", p=128)

    with ExitStack() as es:
        P = Prog(nc, es)
        xaL = [P.sbuf("xa%d" % i, [128, DC, T], F32) for i in range(2)]
        hTL = [P.sbuf("hT%d" % i, [128, DC, T], BF16) for i in range(2)]
        xhL = [P.sbuf("xh%d" % i, [128, DC, 2], F32) for i in range(2)]
        hhL = [P.sbuf("hh%d" % i, [128, DC, 2], BF16) for i in range(2)]
        nsq = [P.sbuf("nsq%d" % i, [128, T], BF16) for i in range(2)]
        nln = P.sbuf("nln", [128, T], F32)
        rstdL = [P.sbuf("rstd%d" % i, [128, T], F32) for i in range(2)]
        KT = P.sbuf("KT", [128, 2, SEQ], BF16)
        V = P.sbuf("V", [128, SEQ // 128, 256], BF16)
        cstL = [P.sbuf("cst%d" % i, [128, 2, T], F32) for i in range(2)]
        xa, hT, xh, hh, cst, rstd = xaL[0], hTL[0], xhL[0], hhL[0], cstL[0], rstdL[0]
        vec = P.sbuf("vec", [128, NVEC], F32)
        con = P.sbuf("con", [128, 2, 128], F32)
        ones_bf = P.sbuf("ones_bf", [128, 128], BF16)
        gb = P.sbuf("gb", [128, 2, 128], F32)
        gmax = P.sbuf("gmax", [128, 2], F32)
        negc = P.sbuf("negc", [128, 1], F32)
        NW = 3
        wt = [P.sbuf("wt%d" % i, [128, DC, 128], BF16) for i in range(NW)]
        wvbA = P.sbuf("wvbA", [128, 8 * 256], BF16)
        wvbB = P.sbuf("wvbB", [128, 8 * 256], BF16)
        wvb3 = [wvbA[:].rearrange("p (k c) -> p k c", k=8), wvbB[:].rearrange("p (k c) -> p k c", k=8)]
        wk = [wt[0], wt[1]]
        convw = [P.sbuf("convw%d" % i, [128, 2], F32) for i in range(3)]
        qT = P.sbuf("qT", [128, 8 * T], BF16)
        oT = P.sbuf("oT", [128, 8 * T], BF16)
        cvT = P.sbuf("cvT", [128, 8 * T], BF16)
        qT3 = qT[:].rearrange("p (h t) -> p h t", h=8)
        oT3 = oT[:].rearrange("p (h t) -> p h t", h=8)
        cvT3 = cvT[:].rearrange("p (h t) -> p h t", h=8)
        mg = P.sbuf("mg", [128, DC, T], BF16)
        pex = [P.sbuf("pex%d" % i, [128, 2 * T], BF16) for i in range(3)]
        qsq = P.sbuf("qsq", [128, T], BF16)
        qln = P.sbuf("qln", [128, T], F32)
        qrs = P.sbuf("qrs", [128, T], F32)
        qn = P.sbuf("qn", [128, T], F32)
        qt1 = P.sbuf("qt1", [128, T], F32)
        qt2 = P.sbuf("qt2", [128, T], F32)
        rl = P.sbuf("rl", [128, T], F32)
        u = P.sbuf("u", [128, T + 2], F32)
        ctmp = P.sbuf("ctmp", [128, T + 2], F32)
        cacc = P.sbuf("cacc", [128, T], F32)
        sga = P.sbuf("sga", [128, T], F32)
        sgc = P.sbuf("sgc", [128, T], F32)
        m1 = P.sbuf("m1", [128, T], F32)
        m2 = P.sbuf("m2", [128, T], F32)
        ptb = P.sbuf("ptb", [128, 2, T], BF16)
        if do_peer:
            kT = P.sbuf("kTs", [128, 16, 128], F32)
            qs = [P.sbuf("qs%d" % i, [128, T], F32) for i in range(2)]
            sct = [P.sbuf("sct%d" % i, [128, 128], F32) for i in range(2)]
            sc2 = P.sbuf("sc2", [128, 128], F32)
            svL = [P.sbuf("sv%d" % i, [128, 16, 16], F32) for i in range(2)]
            siL = [P.sbuf("si%d" % i, [128, 16, 16], U32) for i in range(2)]
            sif = P.sbuf("sif", [128, 16, 16], F32)
            cand = P.sbuf("cand", [128, 256], F32)
            cand2 = P.sbuf("cand2", [128, 256], F32)
            cidx = P.sbuf("cidx", [128, 256], F32)
            junk = P.sbuf("junk", [128, 256], F32)
            tv = P.sbuf("tv", [128, 8, 16], F32)
            eidf = P.sbuf("eidf", [128, 128], F32)
            eidiL = [P.sbuf("eidi%d" % i, [128, 128], U32) for i in range(2)]
            negm = P.sbuf("negm", [128, 8], F32)
            ge = P.sbuf("ge", [128, 8, 16], F32)
            gsum = P.sbuf("gsum", [128, 8], F32)
            rgs = P.sbuf("rgs", [128, 8], F32)
            gate = P.sbuf("gate", [128, 128], F32)
            araw = P.sbuf("araw", [128, 128], F32)
            araw2 = P.sbuf("araw2", [128, 128], F32)
            asc = P.sbuf("asc", [128, 128], F32)
            hid = P.sbuf("hid", [128, 128], F32)
            xg = [P.sbuf("xg%d" % i, [128, 128], F32) for i in range(2)]
            xn_tm = P.sbuf("xn_tm", [128, D], BF16)
            prod = [P.sbuf("prod0", [128, D], BF16)]
            rs_tm = P.sbuf("rs_tm", [128, 128], F32)
            accq = [P.sbuf("accq%d" % i, [128, 512], F32) for i in range(2)]
            NB = 5
            ubuf = [P.sbuf("ubuf%d" % i, [128, D], BF16) for i in range(3)] + [wvbA, wvbB]
            junk2 = P.sbuf("junk2", [128, D], BF16)
            convtok = P.sbuf("convtok", [128, 2], F32)
            convtokv = P.sbuf("convtokv", [128, 2], F32)
            identb = P.sbuf("identb", [128, 128], BF16)
            dg = [P.sbuf("dg%d" % i, [128, 128], BF16) for i in range(4)]
        psb = [P.psum("ps%d" % i, [128, 512], F32) for i in range(8)]
        rot = {"i": 0}

        def ps_next():
            t = psb[4 + rot["i"] % 4]
            rot["i"] += 1
            return t

        rot4 = {"i": 0}

        def ps_next4():
            t = psb[4 + rot4["i"] % 4]
            rot4["i"] += 1
            return t

        ident = con[:, 0, :]
        rmat = con[:, 1, :]

        def dbg_out(name, tile, ap, shape, dt=F32):
            if dbg is None or name not in dbg:
                return
            d = nc.dram_tensor("dbg_" + name, list(shape), dt, kind="ExternalOutput").ap()
            P.dma("sp", lambda e: e.dma_start(out=d, in_=ap), tile, False)
            dbg_outs[name] = tile

        wrot = {"i": 0}

        def wgroup(name, j):
            if name == "win":
                return 0 if j < CGA else 1
            if name in ("wao", "wco"):
                return 1
            return 2

        def load_w(name, j, kc=DC):
            t = wt[wrot["i"] % NW]
            wrot["i"] += 1
            src_ap = wbf[name][j]
            tok = convw[wgroup(name, j)]
            P.dma("sp", lambda e: e.dma_start(out=t[:, 0:kc, :], in_=src_ap), t, True, extra_reads=[tok])
            return t

        def proj(ps, n, wtile, rhs_tile, rhs_fn, kc=DC):
            for k in range(kc):
                P.op("pe", lambda e, k=k: e.matmul(ps[:, 0:n], lhsT=wtile[:, k, :], rhs=rhs_fn(k),
                                                    start=(k == 0), stop=(k == kc - 1)),
                     [wtile, rhs_tile], [ps])

        def norm(x_tile, n, gbase, out_tile, rstd):
            ps = ps_next()
            for dc in range(DC):
                sq = nsq[dc % 2]
                P.op("act", lambda e, dc=dc, sq=sq: e.activation(out=sq[:, 0:n], in_=x_tile[:, dc, 0:n], func=AF.Square),
                     [x_tile], [sq])
                P.op("pe", lambda e, dc=dc, sq=sq: e.matmul(ps[:, 0:n], lhsT=ones_bf[:], rhs=sq[:, 0:n],
                                                            start=(dc == 0), stop=(dc == DC - 1)),
                     [ones_bf, sq], [ps])
            P.op("act", lambda e: e.activation(out=nln[:, 0:n], in_=ps[:, 0:n], func=AF.Ln, scale=1.0 / D, bias=EPS),
                 [ps], [nln])
            P.op("act", lambda e: e.activation(out=rstd[:, 0:n], in_=nln[:, 0:n], func=AF.Exp, scale=-0.5),
                 [nln], [rstd])
            for dc in range(DC):
                P.op("dve", lambda e, dc=dc: e.scalar_tensor_tensor(
                    out=out_tile[:, dc, 0:n], in0=x_tile[:, dc, 0:n], scalar=vec[:, gbase + dc:gbase + dc + 1],
                    in1=rstd[:, 0:n], op0=ALU.mult, op1=ALU.mult), [x_tile, vec, rstd], [out_tile])

        def qkrope(ps, gcol, out_tile, out_ap, cst):
            n = T
            P.op("act", lambda e: e.activation(out=qsq[:, 0:n], in_=ps[:, 0:n], func=AF.Square), [ps], [qsq])
            ps2 = ps_next()
            P.op("pe", lambda e: e.matmul(ps2[:, 0:n], lhsT=ones_bf[:], rhs=qsq[:, 0:n], start=True, stop=True),
                 [ones_bf, qsq], [ps2])
            P.op("act", lambda e: e.activation(out=qln[:, 0:n], in_=ps2[:, 0:n], func=AF.Ln, scale=1.0 / HD, bias=EPS),
                 [ps2], [qln])
            P.op("act", lambda e: e.activation(out=qrs[:, 0:n], in_=qln[:, 0:n], func=AF.Exp, scale=-0.5), [qln], [qrs])
            P.op("dve", lambda e: e.scalar_tensor_tensor(out=qn[:, 0:n], in0=ps[:, 0:n], scalar=vec[:, gcol:gcol + 1],
                                                         in1=qrs[:, 0:n], op0=ALU.mult, op1=ALU.mult),
                 [ps, vec, qrs], [qn])
            ps3 = ps_next()
            P.op("pe", lambda e: e.matmul(ps3[:, 0:n], lhsT=rmat, rhs=qn[:, 0:n], start=True, stop=True),
                 [con, qn], [ps3])
            P.op("dve", lambda e: e.tensor_tensor(out=qt1[:, 0:n], in0=qn[:, 0:n], in1=cst[:, 0, :], op=ALU.mult),
                 [qn, cst], [qt1])
            P.op("dve", lambda e: e.tensor_tensor(out=qt2[:, 0:n], in0=ps3[:, 0:n], in1=cst[:, 1, :], op=ALU.mult),
                 [ps3, cst], [qt2])
            P.op("dve", lambda e: e.tensor_tensor(out=out_ap, in0=qt1[:, 0:n], in1=qt2[:, 0:n], op=ALU.add),
                 [qt1, qt2], [out_tile])

        P.dma("sp", lambda e: e.dma_start(out=vec[:], in_=vecs[:, :]), vec, True)
        P.dma("sp", lambda e: e.dma_start(out=con[:], in_=consts[:, :, :]), con, True)
        P.dma("sp", lambda e: e.dma_start(out=gb[:, 0, :], in_=gqk[0:1, :].partition_broadcast(128)), gb, True)
        P.dma("sp", lambda e: e.dma_start(out=gb[:, 1, :], in_=gqk[1:2, :].partition_broadcast(128)), gb, True)
        P.op("dve", lambda e: e.memset(ones_bf[:], 1.0), [], [ones_bf])
        P.op("dve", lambda e: e.tensor_reduce(out=gmax[:], in_=gb[:], axis=AX.X, op=ALU.max, apply_absolute_value=True),
             [gb], [gmax])
        P.op("dve", lambda e: e.tensor_scalar(out=negc[:], in0=gmax[:, 0:1], scalar1=gmax[:, 1:2], scalar2=-float(np.sqrt(HD)),
                                              op0=ALU.mult, op1=ALU.mult), [gmax], [negc])
        P.dma("pool", lambda e: e.dma_start(out=wvb3[0], in_=wv[:, 0:8, :]), wvbA, True)
        P.dma("pool", lambda e: e.dma_start(out=wvb3[1], in_=wv[:, 8:16, :]), wvbB, True)
        for kvh in range(2):
            P.dma("pool", lambda e, kvh=kvh: e.dma_start(out=wk[kvh][:], in_=win[CK + kvh]), wk[kvh], True)
        if do_peer:
            P.dma("sp", lambda e: e.dma_start(out=kT[:], in_=keysT[:, :, :]), kT, True)

        jobs = []
        order = [("win", j) for j in range(0, 8)] + [("win", j) for j in range(CCH, CGA)]
        order += [("wao", j) for j in range(16)] + [("wco", j) for j in range(16)] + [("win", j) for j in range(CGA, 68)]
        for nm in ("wo", "wpq", "wpg", "wpl"):
            order += [(nm, j) for j in range(16)]
        for (name, j) in order:
            a, n, kc = wsrc[name]
            jobs.append((a[j].rearrange("p k c -> p (k c)"), wbf[name][j].rearrange("p k c -> p (k c)"), kc * 128,
                         convw[wgroup(name, j)]))
        if do_peer:
            P.op("dve", lambda e: e.tensor_copy(out=identb[:], in_=ident), [con], [identb])
            for (src, dst, tk) in ((pu, pu_bf, convtok), (pv, pv_bf, convtokv)):
                for r in range(NEXP // 128):
                    jobs.append((src[r * 128:(r + 1) * 128, :], dst[r * 128:(r + 1) * 128, :], D, tk))
            stg = [ubuf[0], ubuf[1], ubuf[2], xn_tm, junk2]
        else:
            stg = [P.sbuf("stg%d" % i, [128, D], BF16) for i in range(4)]
        LAG = len(stg) // 2

        def cv_load(i):
            src, dst, w, tok = jobs[i]
            st = stg[i % len(stg)]
            P.dma("pool", lambda e: e.dma_start(out=st[:, 0:w], in_=src), st, True)

        def cv_store(i):
            src, dst, w, tok = jobs[i]
            st = stg[i % len(stg)]
            P.dma("pool", lambda e: e.dma_start(out=dst, in_=st[:, 0:w]), tok, True, extra_reads=[st])

        for i in range(len(jobs) + LAG):
            if i < len(jobs):
                cv_load(i)
            if i - LAG >= 0:
                cv_store(i - LAG)

        for ta in range(NTA):
            c0 = ta * T
            P.dma("sp", lambda e, c0=c0: e.dma_start(out=xa[:], in_=xT_v[:, :, c0:c0 + T]), xa, True)
            P.dma("sp", lambda e, c0=c0: e.dma_start(out=cst[:], in_=cs[:, :, c0:c0 + T]), cst, True)
            norm(xa, T, VG_MIX, hT, rstd)
            for kvh in range(2):
                w = wk[kvh]
                ps = ps_next()
                proj(ps, T, w, hT, lambda k: hT[:, k, :])
                qkrope(ps, VG_K, KT, KT[:, kvh, c0:c0 + T], cst)
            for sub in range(T // 128):
                ps = ps_next()
                for k in range(DC):
                    P.op("pe", lambda e, k=k, sub=sub, ps=ps: e.matmul(
                        ps[:, 0:256], lhsT=hT[:, k, sub * 128:(sub + 1) * 128], rhs=wvb3[k // 8][:, k % 8, :],
                        start=(k == 0), stop=(k == DC - 1)), [hT, wvbA, wvbB], [ps])
                vi = ta * (T // 128) + sub
                P.op("act", lambda e, ps=ps, vi=vi: e.activation(out=V[:, vi, :], in_=ps[:, 0:256], func=AF.Copy),
                     [ps], [V])
        dbg_out("KT", KT, KT[:, :, 0:512], [128, 2, 512], BF16)
        dbg_out("V", V, V[:, 0:4, :], [128, 4, 256], BF16)

        psO = psb[0]
        psL = psb[1]
        SCALE = float(HD) ** -0.5

        def bind(par):
            return xaL[par], hTL[par], xhL[par], hhL[par], cstL[par], rstdL[par]

        def mixer_units(tb, par):
            xa, hT, xh, hh, cst, rstd = bind(par)
            units = []

            def unit():

                c0 = tb * T
                P.dma("sp", lambda e, c0=c0: e.dma_start(out=xa[:], in_=xT_v[:, :, c0:c0 + T]), xa, True)
                P.dma("sp", lambda e, c0=c0: e.dma_start(out=cst[:], in_=cs[:, :, c0:c0 + T]), cst, True)
                P.dma("sp", lambda e, tb=tb: e.dma_start(out=xh[:], in_=xhalo[:, tb, :, :]), xh, True)
                norm(xh, 2, VG_MIX, hh, rstd)
                norm(xa, T, VG_MIX, hT, rstd)

            units.append((unit, False, False))

            for h in range(8):
                def unit(h=h):
                    w = load_w("win", CQ + h)
                    ps = ps_next()
                    proj(ps, T, w, hT, lambda k: hT[:, k, :])
                    qkrope(ps, VG_Q, qT, qT3[:, h, :], cst)

                units.append((unit, False, False))

            for h in range(8):
                kvh = h // 4
                NSC = SEQ // 128
                NSP = NSC // 2
                state = {}

                def s_mm(sp, kvh=kvh, h=h):
                    pS = ps_next()
                    for j in range(2):
                        sc = 2 * sp + j
                        P.op("pe", lambda e, pS=pS, sc=sc, j=j: e.matmul(
                            pS[:, j * T:(j + 1) * T], lhsT=KT[:, kvh, sc * 128:(sc + 1) * 128], rhs=qT3[:, h, :],
                            start=True, stop=True), [KT, qT], [pS])
                    return pS

                for part in range(4):
                    def unit(h=h, kvh=kvh, part=part, state=state, s_mm=s_mm, NSP=NSP, NSC=NSC):
                        if part == 0:
                            state["next"] = s_mm(0)
                        for sp in range(part * 4, part * 4 + 4):
                            pS = state["next"]
                            if sp + 1 < NSP:
                                state["next"] = s_mm(sp + 1)
                            px = pex[sp % 3]
                            P.op("act", lambda e, pS=pS, px=px: e.activation(out=px[:], in_=pS[:, 0:2 * T], func=AF.Exp,
                                                                              scale=SCALE, bias=negc[:, 0:1]),
                                 [pS, negc], [px])
                            for j in range(2):
                                sc = 2 * sp + j
                                P.op("pe", lambda e, px=px, sc=sc, kvh=kvh, j=j: e.matmul(
                                    psO[:, 0:T], lhsT=V[:, sc, kvh * 128:(kvh + 1) * 128], rhs=px[:, j * T:(j + 1) * T],
                                    start=(sc == 0), stop=(sc == NSC - 1)), [V, px], [psO])
                                P.op("pe", lambda e, px=px, sc=sc, j=j: e.matmul(
                                    psL[:, 0:T], lhsT=ones_bf[:], rhs=px[:, j * T:(j + 1) * T],
                                    start=(sc == 0), stop=(sc == NSC - 1)), [ones_bf, px], [psL])
                        if part == 3:
                            P.op("dve", lambda e: e.reciprocal(out=rl[:], in_=psL[:, 0:T]), [psL], [rl])
                            P.op("dve", lambda e, h=h: e.tensor_tensor(out=oT3[:, h, :], in0=psO[:, 0:T], in1=rl[:], op=ALU.mult),
                                 [psO, rl], [oT])

                    units.append((unit, True, part > 0))

            for ci in range(8):
                def unit(ci=ci):
                    w_h = load_w("win", CCH + ci)
                    pH = ps_next()
                    proj(pH, T, w_h, hT, lambda k: hT[:, k, :])
                    pHh = ps_next()
                    proj(pHh, 2, w_h, hh, lambda k: hh[:, k, :])
                    w_c = load_w("win", CCC + ci)
                    pC = ps_next()
                    proj(pC, T, w_c, hT, lambda k: hT[:, k, :])
                    pCh = ps_next()
                    proj(pCh, 2, w_c, hh, lambda k: hh[:, k, :])
                    P.op("act", lambda e, pH=pH: e.activation(out=ctmp[:, 1:T + 1], in_=pH[:, 0:T], func=AF.Copy), [pH], [ctmp])
                    P.op("act", lambda e, pHh=pHh: e.activation(out=ctmp[:, 0:1], in_=pHh[:, 0:1], func=AF.Copy), [pHh], [ctmp])
                    P.op("act", lambda e, pHh=pHh: e.activation(out=ctmp[:, T + 1:T + 2], in_=pHh[:, 1:2], func=AF.Copy), [pHh], [ctmp])
                    P.op("dve", lambda e, pC=pC: e.tensor_tensor(out=u[:, 1:T + 1], in0=ctmp[:, 1:T + 1], in1=pC[:, 0:T], op=ALU.mult),
                         [ctmp, pC], [u])
                    P.op("dve", lambda e, pCh=pCh: e.tensor_tensor(out=u[:, 0:1], in0=ctmp[:, 0:1], in1=pCh[:, 0:1], op=ALU.mult),
                         [ctmp, pCh], [u])
                    P.op("dve", lambda e, pCh=pCh: e.tensor_tensor(out=u[:, T + 1:T + 2], in0=ctmp[:, T + 1:T + 2], in1=pCh[:, 1:2], op=ALU.mult),
                         [ctmp, pCh], [u])
                    wc0 = VCW + ci * 3
                    P.op("dve", lambda e, wc0=wc0: e.tensor_scalar(out=cacc[:], in0=u[:, 0:T], scalar1=vec[:, wc0:wc0 + 1], scalar2=None,
                                                                    op0=ALU.mult), [u, vec], [cacc])
                    for kk in (1, 2):
                        P.op("dve", lambda e, wc0=wc0, kk=kk: e.scalar_tensor_tensor(
                            out=cacc[:], in0=u[:, kk:kk + T], scalar=vec[:, wc0 + kk:wc0 + kk + 1], in1=cacc[:],
                            op0=ALU.mult, op1=ALU.add), [u, vec, cacc], [cacc])
                    w_b = load_w("win", CCB + ci)
                    pB = ps_next()
                    proj(pB, T, w_b, hT, lambda k: hT[:, k, :])
                    P.op("dve", lambda e, pB=pB, ci=ci: e.tensor_tensor(out=cvT3[:, ci, :], in0=cacc[:], in1=pB[:, 0:T], op=ALU.mult),
                         [cacc, pB], [cvT])

                units.append((unit, False, False))

            for mc in range(DC):
                def unit(mc=mc):
                    w_a = load_w("wao", mc, kc=8)
                    pA = ps_next()
                    proj(pA, T, w_a, oT, lambda k: oT3[:, k, :], kc=8)
                    w_c = load_w("wco", mc, kc=8)
                    pC = ps_next()
                    proj(pC, T, w_c, cvT, lambda k: cvT3[:, k, :], kc=8)
                    w_ga = load_w("win", CGA + mc)
                    pGA = ps_next()
                    proj(pGA, T, w_ga, hT, lambda k: hT[:, k, :])
                    w_gc = load_w("win", CGC + mc)
                    pGC = ps_next()
                    proj(pGC, T, w_gc, hT, lambda k: hT[:, k, :])
                    P.op("act", lambda e, pGA=pGA: e.activation(out=sga[:], in_=pGA[:, 0:T], func=AF.Sigmoid), [pGA], [sga])
                    P.op("act", lambda e, pGC=pGC: e.activation(out=sgc[:], in_=pGC[:, 0:T], func=AF.Sigmoid), [pGC], [sgc])
                    P.op("dve", lambda e, pA=pA: e.tensor_tensor(out=m1[:], in0=sga[:], in1=pA[:, 0:T], op=ALU.mult), [sga, pA], [m1])
                    P.op("dve", lambda e, pC=pC: e.tensor_tensor(out=m2[:], in0=sgc[:], in1=pC[:, 0:T], op=ALU.mult), [sgc, pC], [m2])
                    P.op("dve", lambda e, mc=mc: e.tensor_tensor(out=mg[:, mc, :], in0=m1[:], in1=m2[:], op=ALU.add), [m1, m2], [mg])

                units.append((unit, False, False))

            for oc in range(DC):
                def unit(oc=oc):
                    w = load_w("wo", oc)
                    ps = ps_next()
                    proj(ps, T, w, mg, lambda k: mg[:, k, :])
                    P.op("dve", lambda e, ps=ps, oc=oc: e.tensor_tensor(out=xa[:, oc, :], in0=xa[:, oc, :], in1=ps[:, 0:T], op=ALU.add),
                         [xa, ps], [xa])

                units.append((unit, False, False))

            return units


        def pre_steps(par, sub):
            xa, hT, xh, hh, cst, rstd = bind(par)
            s0 = sub * 128
            sv, si, eidi = svL[sub], siL[sub], eidiL[sub]
            steps = []

            def st():
                pR = ps_next()
                P.op("pe", lambda e, pR=pR: e.transpose(pR[:, 0:128], rstd[:, s0:s0 + 128], ident), [rstd, con], [pR])
                P.op("act", lambda e, pR=pR: e.activation(out=rs_tm[:], in_=pR[:, 0:128], func=AF.Copy), [pR], [rs_tm])
            steps.append(st)
            for dc in range(DC):
                def st(dc=dc):
                    xgt = xg[dc % 2]
                    P.op("dve", lambda e, xgt=xgt: e.tensor_scalar(
                        out=xgt[:], in0=xa[:, dc, s0:s0 + 128], scalar1=vec[:, VG_FFN + dc:VG_FFN + dc + 1], scalar2=None,
                        op0=ALU.mult), [xa, vec], [xgt])
                    pX = ps_next()
                    P.op("pe", lambda e, pX=pX, xgt=xgt: e.transpose(pX[:, 0:128], xgt[:], ident), [xgt, con], [pX])
                    P.op("act", lambda e, pX=pX: e.activation(out=xn_tm[:, dc * 128:(dc + 1) * 128], in_=pX[:, 0:128], func=AF.Copy),
                         [pX], [xn_tm])
                steps.append(st)

            def st():
                P.op("dve", lambda e: e.tensor_copy(out=sif[:], in_=si[:]), [si], [sif])
            steps.append(st)
            for h in range(8):
                def st(h=h):
                    a0 = sv[:, 2 * h, :].unsqueeze(2).to_broadcast([128, 16, 16])
                    a1 = sv[:, 2 * h + 1, :].unsqueeze(1).to_broadcast([128, 16, 16])
                    i0 = sif[:, 2 * h, :].unsqueeze(2).to_broadcast([128, 16, 16])
                    i1 = sif[:, 2 * h + 1, :].unsqueeze(1).to_broadcast([128, 16, 16])
                    cv3 = cand[:].rearrange("p (a b) -> p a b", a=16)
                    ci3 = cidx[:].rearrange("p (a b) -> p a b", a=16)
                    P.op("dve", lambda e: e.tensor_tensor(out=cv3, in0=a0, in1=a1, op=ALU.add), [sv], [cand])
                    P.op("dve", lambda e: e.scalar_tensor_tensor(out=ci3, in0=i0, scalar=128.0, in1=i1,
                                                                 op0=ALU.mult, op1=ALU.add), [sif], [cidx])
                    P.op("dve", lambda e: e.max(out=tv[:, h, 0:8], in_=cand[:]), [cand], [tv])
                    P.op("dve", lambda e: e.match_replace(out=cand2[:], in_to_replace=tv[:, h, 0:8], in_values=cand[:], imm_value=-1e30),
                         [cand, tv], [cand2])
                    P.op("dve", lambda e: e.max(out=tv[:, h, 8:16], in_=cand2[:]), [cand2], [tv])
                steps.append(st)
                for kq in range(4):
                    def st(h=h, kq=kq):
                        for k in range(kq * 4, kq * 4 + 4):
                            P.op("dve", lambda e, k=k: e.scalar_tensor_tensor(
                                out=junk[:], in0=cand[:], scalar=tv[:, h, k:k + 1], in1=cidx[:], op0=ALU.is_equal, op1=ALU.mult,
                                accum_out=eidf[:, h * 16 + k:h * 16 + k + 1]), [cand, tv, cidx], [], nowait=[eidf])
                    steps.append(st)

            def st():
                P.op("dve", lambda e: e.tensor_scalar(out=eidi[:], in0=eidf[:], scalar1=float(NEXP - 1), scalar2=0.0,
                                                      op0=ALU.min, op1=ALU.max), [eidf], [eidi])
                P.op("dve", lambda e: e.tensor_scalar(out=negm[:], in0=tv[:, :, 0], scalar1=-1.0, scalar2=None, op0=ALU.mult), [tv], [negm])
                for h in range(8):
                    P.op("act", lambda e, h=h: e.activation(out=ge[:, h, :], in_=tv[:, h, :], func=AF.Exp, bias=negm[:, h:h + 1],
                                                            accum_out=gsum[:, h:h + 1]), [tv, negm], [ge, gsum])
                P.op("dve", lambda e: e.reciprocal(out=rgs[:], in_=gsum[:]), [gsum], [rgs])
                P.op("dve", lambda e: e.tensor_tensor(out=gate[:].rearrange("p (h k) -> p h k", h=8), in0=ge[:],
                                                      in1=rgs[:].unsqueeze(2).to_broadcast([128, 8, 16]), op=ALU.mult), [ge, rgs], [gate])
            steps.append(st)
            return steps

        def peer_front(par):
            xa, hT, xh, hh, cst, rstd = bind(par)
            steps = []

            def st():
                norm(xa, T, VG_FFN, hT, rstd)
            steps.append(st)
            for hp in range(16):
                def st(hp=hp):
                    w = load_w("wpq", hp)
                    pQ = ps_next()
                    proj(pQ, T, w, hT, lambda k: hT[:, k, :])
                    q_s = qs[hp % 2]
                    P.op("act", lambda e, pQ=pQ, q_s=q_s: e.activation(out=q_s[:], in_=pQ[:, 0:T], func=AF.Copy), [pQ], [q_s])
                    for sub in range(2):
                        sv, si = svL[sub], siL[sub]
                        pS = ps_next()
                        P.op("pe", lambda e, pS=pS, q_s=q_s, sub=sub: e.matmul(
                            pS[:, 0:128], lhsT=q_s[:, sub * 128:(sub + 1) * 128], rhs=kT[:, hp, :], start=True, stop=True),
                            [q_s, kT], [pS])
                        sc_t = sct[sub]
                        P.op("act", lambda e, pS=pS, sc_t=sc_t: e.activation(out=sc_t[:], in_=pS[:, 0:128], func=AF.Copy), [pS], [sc_t])
                        P.op("dve", lambda e, sc_t=sc_t, sv=sv: e.max(out=sv[:, hp, 0:8], in_=sc_t[:]), [sc_t], [sv])
                        P.op("dve", lambda e, sc_t=sc_t, sv=sv, si=si: e.max_index(out=si[:, hp, 0:8], in_max=sv[:, hp, 0:8], in_values=sc_t[:]),
                             [sc_t, sv], [si])
                        P.op("dve", lambda e, sc_t=sc_t, sv=sv: e.match_replace(out=sc2[:], in_to_replace=sv[:, hp, 0:8], in_values=sc_t[:],
                                                                                imm_value=-1e30), [sc_t, sv], [sc2])
                        P.op("dve", lambda e, sv=sv: e.max(out=sv[:, hp, 8:16], in_=sc2[:]), [sc2], [sv])
                        P.op("dve", lambda e, sv=sv, si=si: e.max_index(out=si[:, hp, 8:16], in_max=sv[:, hp, 8:16], in_values=sc2[:]),
                             [sc2, sv], [si])
                steps.append(st)
            return steps + pre_steps(par, 0)

        def peer_body(tb, par, pull, finish_open_head, drain, next_front):
            xa, hT, xh, hh, cst, rstd = bind(par)
            hidden = pre_steps(par, 1)
            for sub in range(T // 128):
                s0 = sub * 128
                eidi = eidiL[sub]
                if sub == 1:
                    for st in hidden:
                        st()
                    hidden = []
                P.op("dve", lambda e: e.memset(araw[:], 0.0), [], [araw])
                P.op("dve", lambda e: e.memset(araw2[:], 0.0), [], [araw2])
                def u_consume(hk):
                    ub = ubuf[hk % NB]
                    if hk % 3 != 0:
                        P.op("dve", lambda e, ub=ub, hk=hk: e.scalar_tensor_tensor(
                            out=junk2[:], in0=ub[:], scalar=1.0, in1=xn_tm[:], op0=ALU.mult, op1=ALU.mult,
                            accum_out=araw[:, hk:hk + 1]), [ub, xn_tm], [], nowait=[araw])
                    else:
                        pr = prod[0]
                        P.op("dve", lambda e, ub=ub, pr=pr: e.tensor_tensor(out=pr[:], in0=ub[:], in1=xn_tm[:], op=ALU.mult),
                             [ub, xn_tm], [pr])
                        P.op("act", lambda e, pr=pr, hk=hk: e.activation(out=pr[:], in_=pr[:], func=AF.Copy,
                                                                          accum_out=araw2[:, hk:hk + 1]), [pr], [pr], nowait=[araw2])

                for hk in range(128 + CLAG):
                    if hk < 128:
                        ub = ubuf[hk % NB]
                        P.dma("pool", lambda e, ub=ub, hk=hk, eidi=eidi: e.indirect_dma_start(
                            out=ub[:], out_offset=None, in_=pu_bf[:, :],
                            in_offset=bass.IndirectOffsetOnAxis(ap=eidi[:, hk:hk + 1], axis=0)), ub, True, extra_reads=[eidi, convtok])
                    if hk % PULL_EVERY == 0:
                        pull(True)
                    if hk - CLAG >= 0:
                        u_consume(hk - CLAG)
                finish_open_head()
                P.op("dve", lambda e: e.tensor_tensor(out=araw[:], in0=araw[:], in1=araw2[:], op=ALU.add), [araw, araw2], [araw])
                P.op("dve", lambda e: e.tensor_scalar(out=asc[:], in0=araw[:], scalar1=rs_tm[:, 0:1], scalar2=None, op0=ALU.mult),
                     [araw, rs_tm], [asc])
                P.op("act", lambda e: e.activation(out=hid[:], in_=asc[:], func=AF.Gelu), [asc], [hid])
                P.op("dve", lambda e: e.tensor_tensor(out=hid[:], in0=hid[:], in1=gate[:], op=ALU.mult), [hid, gate], [hid])
                if sub == 1:
                    drain()
                psV = [psb[0], psb[1], psb[2], psb[3]]
                def v_consume(hk):
                    ub = ubuf[hk % NB]
                    dgt = dg[hk % 4]
                    P.op("dve", lambda e, dgt=dgt, hk=hk: e.tensor_scalar(out=dgt[:], in0=identb[:], scalar1=hid[:, hk:hk + 1], scalar2=None,
                                                                          op0=ALU.mult), [identb, hid], [dgt])
                    for dq in range(4):
                        P.op("pe", lambda e, dgt=dgt, ub=ub, dq=dq, hk=hk: e.matmul(
                            psV[dq][:, 0:512], lhsT=dgt[:], rhs=ub[:, dq * 512:(dq + 1) * 512],
                            start=(hk == 0), stop=(hk == 127)), [dgt, ub], [psV[dq]])

                for hk in range(128 + CLAG):
                    if hk < 128:
                        ub = ubuf[hk % NB]
                        P.dma("pool", lambda e, ub=ub, hk=hk, eidi=eidi: e.indirect_dma_start(
                            out=ub[:], out_offset=None, in_=pv_bf[:, :],
                            in_offset=bass.IndirectOffsetOnAxis(ap=eidi[:, hk:hk + 1], axis=0)), ub, True, extra_reads=[eidi, convtokv])
                    if hk - CLAG >= 0:
                        v_consume(hk - CLAG)
                    if sub == 0 and hidden and hk % 2 == 1:
                        hidden.pop(0)()
                    if sub == 1 and next_front and hk >= 2:
                        next_front.pop(0)()
                if tb == 0 and sub == 0:
                    dbg_out("eidf", eidf, eidf[:], [128, 128], F32)
                    dbg_out("gate", gate, gate[:], [128, 128], F32)
                    dbg_out("asc", asc, asc[:], [128, 128], F32)
                for dq in range(4):
                    aq = accq[dq % 2]
                    P.op("act", lambda e, dq=dq, aq=aq: e.activation(out=aq[:], in_=psV[dq][:, 0:512], func=AF.Copy), [psV[dq]], [aq])
                    for j in range(4):
                        dc = dq * 4 + j
                        pX = ps_next4()
                        P.op("pe", lambda e, pX=pX, j=j, aq=aq: e.transpose(pX[:, 0:128], aq[:, j * 128:(j + 1) * 128], ident), [aq, con], [pX])
                        P.op("dve", lambda e, pX=pX, dc=dc, s0=s0: e.tensor_tensor(
                            out=xa[:, dc, s0:s0 + 128], in0=xa[:, dc, s0:s0 + 128], in1=pX[:, 0:128], op=ALU.add), [xa, pX], [xa])


        def ple_units(tb, par):
            xa, hT, xh, hh, cst, rstd = bind(par)
            c0 = tb * T
            units = []

            def unit():
                norm(xa, T, VG_PLE, hT, rstd)
                P.dma("pool", lambda e, c0=c0: e.dma_start(out=ptb[:], in_=pT_v[:, :, c0:c0 + T]), ptb, True)

            units.append((unit, False, False))
            for oc in range(DC):
                def unit(oc=oc):
                    w_g = load_w("wpg", oc)
                    pG = ps_next()
                    proj(pG, T, w_g, hT, lambda k: hT[:, k, :])
                    w_p = load_w("wpl", oc, kc=2)
                    pP = ps_next()
                    proj(pP, T, w_p, ptb, lambda k: ptb[:, k, :], kc=2)
                    P.op("act", lambda e, pG=pG: e.activation(out=sga[:], in_=pG[:, 0:T], func=AF.Sigmoid), [pG], [sga])
                    P.op("dve", lambda e, pP=pP: e.tensor_tensor(out=m1[:], in0=sga[:], in1=pP[:, 0:T], op=ALU.mult), [sga, pP], [m1])
                    P.op("dve", lambda e, oc=oc: e.tensor_tensor(out=xa[:, oc, :], in0=xa[:, oc, :], in1=m1[:], op=ALU.add), [xa, m1], [xa])
                    if oc == DC - 1:
                        P.dma("sp", lambda e, c0=c0: e.dma_start(out=outT_v[:, :, c0:c0 + T], in_=xa[:]), xa, False)

                units.append((unit, False, False))
            return units

        PULL_EVERY = 2
        CLAG = 3
        pending = {"units": [], "i": 0}

        def pull(allow01):
            u, i = pending["units"], pending["i"]
            if i < len(u) and (allow01 or not u[i][1]):
                pending["i"] = i + 1
                u[i][0]()

        def finish_open_head():
            u = pending["units"]
            while pending["i"] < len(u) and u[pending["i"]][2]:
                pull(True)

        def drain():
            while pending["i"] < len(pending["units"]):
                pull(True)

        for (fn, _a, _b) in mixer_units(0, 0):
            fn()
        carry = []
        front = peer_front(0) if do_peer else []
        for tb in range(nt_run):
            par = tb % 2
            pending["units"] = carry + (mixer_units(tb + 1, 1 - par) if tb + 1 < nt_run else [])
            pending["i"] = 0
            if do_peer:
                for st in front:
                    st()
                nxt = peer_front(1 - par) if tb + 1 < nt_run else []
                peer_body(tb, par, pull, finish_open_head, drain, nxt)
                front = nxt
            drain()
            if tb == 0:
                dbg_out("x2", xaL[0], xaL[0][:], [128, DC, T], F32)
            carry = ple_units(tb, par)
        for (fn, _a, _b) in carry:
            fn()

        P.final_wait("sp", xaL + list(dbg_outs.values()))
        P.emit()
        n_ops = P.n_ops
    return nc, n_ops


def _lay(W):
    K, N = W.shape
    return np.ascontiguousarray(W.reshape(K // 128, 128, N // 128, 128).transpose(2, 1, 0, 3))


def _rope_tables():
    S = SEQ
    rows = S // 64
    row = np.repeat(np.arange(rows, dtype=np.int32), 64)[:S]
    col = np.tile(np.arange(64, dtype=np.int32), rows)
    inv = (np.float32(10000.0) ** (-np.arange(0, 64, 2, dtype=np.float32) / np.float32(64))).astype(np.float32)
    ang_r = row.astype(np.float32)[:, None] * inv[None, :]
    ang_c = col.astype(np.float32)[:, None] * inv[None, :]
    ang = np.concatenate([ang_r, ang_r, ang_c, ang_c], axis=-1)
    return np.cos(ang).astype(np.float32), np.sin(ang).astype(np.float32)


def _consts():
    c = np.zeros((128, 2, 128), np.float32)
    c[:, 0, :] = np.eye(128, dtype=np.float32)
    for a in range(2):
        for f in range(32):
            i0 = a * 64 + f
            i1 = a * 64 + 32 + f
            c[i1, 1, i0] = -1.0
            c[i0, 1, i1] = 1.0
    return c


def prepare_inputs(x, p, g_mix, w_in, g_q, g_k, conv_w, w_attn_out, w_conv_out, w_out,
                   g_ffn, w_peer_q, peer_sub_keys, peer_u, peer_v, g_ple, w_ple, w_ple_gate):
    f = lambda a: np.asarray(a, dtype=np.float32)
    x = f(x); p = f(p)[0]
    w_in = f(w_in)[0]
    shared = {}
    shared["win"] = _lay(w_in)
    shared["wv"] = np.ascontiguousarray(w_in[:, 1280:1536].reshape(16, 128, 256).transpose(1, 0, 2))
    shared["wao"] = _lay(f(w_attn_out)[0])
    shared["wco"] = _lay(f(w_conv_out)[0])
    shared["wo"] = _lay(f(w_out)[0])
    shared["wpq"] = _lay(f(w_peer_q)[0])
    shared["wpg"] = _lay(f(w_ple_gate)[0])
    shared["wpl"] = _lay(f(w_ple)[0])
    sk = f(peer_sub_keys)[0].reshape(16, 128, 128)
    shared["keysT"] = np.ascontiguousarray(sk.transpose(2, 0, 1))
    shared["pu"] = np.ascontiguousarray(f(peer_u)[0])
    shared["pv"] = np.ascontiguousarray(f(peer_v)[0])
    vecs = np.zeros((128, NVEC), np.float32)
    vecs[:, VG_MIX:VG_MIX + 16] = f(g_mix)[0].reshape(16, 128).T
    vecs[:, VG_FFN:VG_FFN + 16] = f(g_ffn)[0].reshape(16, 128).T
    vecs[:, VG_PLE:VG_PLE + 16] = f(g_ple)[0].reshape(16, 128).T
    vecs[:, VG_Q] = f(g_q)[0]
    vecs[:, VG_K] = f(g_k)[0]
    cw = f(conv_w)[0]
    vecs[:, VCW:VCW + 24] = cw.reshape(3, 8, 128).transpose(2, 1, 0).reshape(128, 24)
    shared["vecs"] = vecs
    shared["gqk"] = np.stack([f(g_q)[0], f(g_k)[0]], axis=0)
    shared["consts"] = _consts()
    cos, sin = _rope_tables()
    in_maps = []
    for c in range(8):
        b, half = c // 2, c % 2
        own = slice(half * OWN, (half + 1) * OWN)
        oth = slice((1 - half) * OWN, (2 - half) * OWN)
        xb = x[b]
        m = dict(shared)
        m["xT"] = np.ascontiguousarray(np.concatenate([xb[own], xb[oth]], axis=0).T)
        xh = np.zeros((NT, 2, D), np.float32)
        for tb in range(NT):
            l = half * OWN + tb * T - 1
            r = half * OWN + (tb + 1) * T
            if l >= 0:
                xh[tb, 0] = xb[l]
            if r < SEQ:
                xh[tb, 1] = xb[r]
        m["xhalo"] = np.ascontiguousarray(xh.reshape(NT, 2, DC, 128).transpose(3, 0, 2, 1))
        m["pT"] = np.ascontiguousarray(p[b, own].T)
        cso = np.stack([np.concatenate([cos[own], cos[oth]], axis=0).T,
                        np.concatenate([sin[own], sin[oth]], axis=0).T], axis=1)
        m["cs"] = np.ascontiguousarray(cso)
        in_maps.append(m)
    return in_maps


_CACHE = {}


def kernel(**inputs):
    in_maps = prepare_inputs(**inputs)
    if "nc" not in _CACHE:
        _CACHE["nc"] = build_program()[0]
    nc = _CACHE["nc"]
    res = run_bass_kernel_spmd(nc, in_maps, core_ids=list(range(8)))
    out = np.empty((4, SEQ, D), np.float32)
    for c in range(8):
        b, half = c // 2, c % 2
        out[b, half * OWN:(half + 1) * OWN, :] = np.asarray(res.results[c]["outT"]).T
    return out
```

```python
import os
import numpy as np
from contextlib import ExitStack
import concourse.bass as bass
import concourse.mybir as mybir
from concourse.bass_utils import run_bass_kernel_spmd

F32 = mybir.dt.float32
BF16 = mybir.dt.bfloat16
U32 = mybir.dt.uint32
I32 = mybir.dt.int32
AF = mybir.ActivationFunctionType
ALU = mybir.AluOpType
AX = mybir.AxisListType

ENGS = ["pe", "act", "dve", "pool", "sp"]
NOSELF = set(os.environ.get("K_NOSELF", "").split(",")) - {""}


class Tile:
    def __init__(self, prog, handle, name):
        self.prog = prog
        self.h = handle
        self.name = name
        self.wev = None
        self.revs = []
        self.dsem = None
        self.dcnt = 0

    def __getitem__(self, idx):
        return self.h[idx]


class Prog:
    def __init__(self, nc, es):
        self.nc = nc
        self.es = es
        self.q = {e: [] for e in ENGS}
        self.cnt = {e: 0 for e in ENGS}
        self.sem = {}
        for e in ["pe", "act", "dve", "pool"]:
            self.sem[e] = es.enter_context(nc.semaphore("s_" + e))
        self.seen = {e: {} for e in ENGS}
        self.n_ops = 0

    def sbuf(self, name, shape, dt):
        h = self.es.enter_context(self.nc.sbuf_tensor(name, list(shape), dt))
        return Tile(self, h, name)

    def psum(self, name, shape, dt):
        h = self.es.enter_context(self.nc.psum_tensor(name, list(shape), dt))
        return Tile(self, h, name)

    def _dsem(self, t):
        if t.dsem is None:
            t.dsem = self.es.enter_context(self.nc.semaphore("d_" + t.name))
        return t.dsem

    def _collect(self, eng, reads, writes, is_dma):
        waits = []
        for t in reads:
            if t.wev is not None:
                waits.append(t.wev)
        for t in writes:
            if t.wev is not None:
                if not (is_dma and t.wev[3]):
                    waits.append(t.wev)
            waits.extend(t.revs)
        out = []
        seen = self.seen[eng]
        for (sem, val, weng, wdma) in waits:
            if eng == "pe" and weng == "pe" and not wdma:
                continue
            if weng == eng and not wdma and eng in NOSELF:
                continue
            key = id(sem)
            if seen.get(key, 0) >= val:
                continue
            seen[key] = val
            out.append((sem, val))
        return out

    def op(self, eng, fn, reads=(), writes=(), nowait=()):
        reads = [t for t in reads if t is not None]
        writes = [t for t in writes if t is not None]
        waits = self._collect(eng, reads, writes, False)
        writes = writes + list(nowait)
        self.cnt[eng] += 1
        ev = (self.sem[eng], self.cnt[eng], eng, False)
        self.q[eng].append((waits, fn, (self.sem[eng], 1)))
        for t in writes:
            t.wev = ev
            t.revs = []
        for t in reads:
            if t not in writes:
                t.revs = [r for r in t.revs if r[0] is not ev[0]] + [ev]
        self.n_ops += 1

    def dma(self, eng, fn, tile, is_write, extra_reads=()):
        reads = list(extra_reads) + ([] if is_write else [tile])
        writes = [tile] if is_write else []
        waits = self._collect(eng, reads, writes, True)
        sem = self._dsem(tile)
        tile.dcnt += 16
        ev = (sem, tile.dcnt, eng, True)
        self.q[eng].append((waits, fn, (sem, 16)))
        if is_write:
            tile.wev = ev
            tile.revs = []
        else:
            tile.revs = [r for r in tile.revs if r[0] is not sem] + [ev]
        for t in extra_reads:
            t.revs = [r for r in t.revs if r[0] is not sem] + [ev]
        self.n_ops += 1

    def final_wait(self, eng, tiles):
        waits = []
        for t in tiles:
            waits.extend(t.revs)
            if t.wev is not None:
                waits.append(t.wev)
        best = {}
        for (sem, val, _e, _d) in waits:
            k = id(sem)
            if k not in best or best[k][1] < val:
                best[k] = (sem, val)
        self.q[eng].append((list(best.values()), None, None))

    def emit(self):
        nc = self.nc

        def replay(ename, e):
            for (waits, fn, inc) in self.q[ename]:
                for (sem, val) in waits:
                    e.wait_ge(sem, val)
                if fn is not None:
                    ins = fn(e)
                    ins.then_inc(inc[0], inc[1])

        with nc.Block() as block:
            @block.tensor
            def _(e):
                replay("pe", e)

            @block.scalar
            def _(e):
                replay("act", e)

            @block.vector
            def _(e):
                replay("dve", e)

            @block.gpsimd
            def _(e):
                replay("pool", e)

            @block.sync
            def _(e):
                replay("sp", e)


D = 2048
DC = 16
SEQ = 4096
OWN = 2048
T = 256
NT = OWN // T
NTA = SEQ // T
HD = 128
NEXP = 16384
EPS = 1e-6
CQ, CK, CCH, CCB, CCC, CGA, CGC = 0, 8, 12, 20, 28, 36, 52
VG_MIX, VG_FFN, VG_PLE, VG_Q, VG_K, VCW = 0, 16, 32, 48, 49, 50
NVEC = 80


def build_program(nt_run=NT, do_peer=True, dbg=None):
    nc = bass.Bass("TRN2", target_bir_lowering=False)

    def din(name, shape, dt=F32):
        return nc.dram_tensor(name, list(shape), dt, kind="ExternalInput").ap()

    xT = din("xT", [D, SEQ])
    xhalo = din("xhalo", [128, NT, DC, 2])
    pT = din("pT", [256, OWN])
    cs = din("cs", [128, 2, SEQ])
    vecs = din("vecs", [128, NVEC])
    gqk = din("gqk", [2, 128])
    consts = din("consts", [128, 2, 128])
    win = din("win", [68, 128, DC, 128])
    wv = din("wv", [128, DC, 256])
    wao = din("wao", [16, 128, 8, 128])
    wco = din("wco", [16, 128, 8, 128])
    wo = din("wo", [16, 128, DC, 128])
    wpq = din("wpq", [16, 128, DC, 128])
    wpg = din("wpg", [16, 128, DC, 128])
    wpl = din("wpl", [16, 128, 2, 128])
    keysT = din("keysT", [128, 16, 128])
    pu = din("pu", [NEXP, D])
    pv = din("pv", [NEXP, D])
    outT = nc.dram_tensor("outT", [D, OWN], F32, kind="ExternalOutput").ap()
    pu_bf = nc.dram_tensor("pu_bf", [NEXP, D], BF16, kind="Internal").ap()
    pv_bf = nc.dram_tensor("pv_bf", [NEXP, D], BF16, kind="Internal").ap()
    wsrc = {"win": (win, 68, DC), "wao": (wao, 16, 8), "wco": (wco, 16, 8), "wo": (wo, 16, DC),
            "wpq": (wpq, 16, DC), "wpg": (wpg, 16, DC), "wpl": (wpl, 16, 2)}
    wbf = {k: nc.dram_tensor(k + "_bf", [n, 128, kc, 128], BF16, kind="Internal").ap() for k, (a, n, kc) in wsrc.items()}
    dbg_outs = {}

    xT_v = xT.rearrange("(dc p) t -> p dc t", p=128)
    outT_v = outT.rearrange("(dc p) t -> p dc t", p=128)
    pT_v = pT.rearrange("(kc p) t -> p kc t", p=128)

    with ExitStack() as es:
        P = Prog(nc, es)
        xaL = [P.sbuf("xa%d" % i, [128, DC, T], F32) for i in range(2)]
        hTL = [P.sbuf("hT%d" % i, [128, DC, T], BF16) for i in range(2)]
        xhL = [P.sbuf("xh%d" % i, [128, DC, 2], F32) for i in range(2)]
        hhL = [P.sbuf("hh%d" % i, [128, DC, 2], BF16) for i in range(2)]
        nsq = [P.sbuf("nsq%d" % i, [128, T], BF16) for i in range(2)]
        nln = P.sbuf("nln", [128, T], F32)
        rstdL = [P.sbuf("rstd%d" % i, [128, T], F32) for i in range(2)]
        KT = P.sbuf("KT", [128, 2, SEQ], BF16)
        V = P.sbuf("V", [128, SEQ // 128, 256], BF16)
        cstL = [P.sbuf("cst%d" % i, [128, 2, T], F32) for i in range(2)]
        xa, hT, xh, hh, cst, rstd = xaL[0], hTL[0], xhL[0], hhL[0], cstL[0], rstdL[0]
        vec = P.sbuf("vec", [128, NVEC], F32)
        con = P.sbuf("con", [128, 2, 128], F32)
        ones_bf = P.sbuf("ones_bf", [128, 128], BF16)
        gb = P.sbuf("gb", [128, 2, 128], F32)
        gmax = P.sbuf("gmax", [128, 2], F32)
        negc = P.sbuf("negc", [128, 1], F32)
        NW = 3
        wt = [P.sbuf("wt%d" % i, [128, DC, 128], BF16) for i in range(NW)]
        wvbA = P.sbuf("wvbA", [128, 8 * 256], BF16)
        wvbB = P.sbuf("wvbB", [128, 8 * 256], BF16)
        wvb3 = [wvbA[:].rearrange("p (k c) -> p k c", k=8), wvbB[:].rearrange("p (k c) -> p k c", k=8)]
        wk = [wt[0], wt[1]]
        convw = [P.sbuf("convw%d" % i, [128, 2], F32) for i in range(3)]
        qT = P.sbuf("qT", [128, 8 * T], BF16)
        oT = P.sbuf("oT", [128, 8 * T], BF16)
        cvT = P.sbuf("cvT", [128, 8 * T], BF16)
        qT3 = qT[:].rearrange("p (h t) -> p h t", h=8)
        oT3 = oT[:].rearrange("p (h t) -> p h t", h=8)
        cvT3 = cvT[:].rearrange("p (h t) -> p h t", h=8)
        mg = P.sbuf("mg", [128, DC, T], BF16)
        pex = [P.sbuf("pex%d" % i, [128, 2 * T], BF16) for i in range(3)]
        qsq = P.sbuf("qsq", [128, T], BF16)
        qln = P.sbuf("qln", [128, T], F32)
        qrs = P.sbuf("qrs", [128, T], F32)
        qn = P.sbuf("qn", [128, T], F32)
        qt1 = P.sbuf("qt1", [128, T], F32)
        qt2 = P.sbuf("qt2", [128, T], F32)
        rl = P.sbuf("rl", [128, T], F32)
        u = P.sbuf("u", [128, T + 2], F32)
        ctmp = P.sbuf("ctmp", [128, T + 2], F32)
        cacc = P.sbuf("cacc", [128, T], F32)
        sga = P.sbuf("sga", [128, T], F32)
        sgc = P.sbuf("sgc", [128, T], F32)
        m1 = P.sbuf("m1", [128, T], F32)
        m2 = P.sbuf("m2", [128, T], F32)
        ptb = P.sbuf("ptb", [128, 2, T], BF16)
        if do_peer:
            kT = P.sbuf("kTs", [128, 16, 128], F32)
            qs = [P.sbuf("qs%d" % i, [128, T], F32) for i in range(2)]
            sct = [P.sbuf("sct%d" % i, [128, 128], F32) for i in range(2)]
            sc2 = P.sbuf("sc2", [128, 128], F32)
            svL = [P.sbuf("sv%d" % i, [128, 16, 16], F32) for i in range(2)]
            siL = [P.sbuf("si%d" % i, [128, 16, 16], U32) for i in range(2)]
            sif = P.sbuf("sif", [128, 16, 16], F32)
            cand = P.sbuf("cand", [128, 256], F32)
            cand2 = P.sbuf("cand2", [128, 256], F32)
            cidx = P.sbuf("cidx", [128, 256], F32)
            junk = P.sbuf("junk", [128, 256], F32)
            tv = P.sbuf("tv", [128, 8, 16], F32)
            eidf = P.sbuf("eidf", [128, 128], F32)
            eidiL = [P.sbuf("eidi%d" % i, [128, 128], U32) for i in range(2)]
            negm = P.sbuf("negm", [128, 8], F32)
            ge = P.sbuf("ge", [128, 8, 16], F32)
            gsum = P.sbuf("gsum", [128, 8], F32)
            rgs = P.sbuf("rgs", [128, 8], F32)
            gate = P.sbuf("gate", [128, 128], F32)
            araw = P.sbuf("araw", [128, 128], F32)
            araw2 = P.sbuf("araw2", [128, 128], F32)
            asc = P.sbuf("asc", [128, 128], F32)
            hid = P.sbuf("hid", [128, 128], F32)
            xg = [P.sbuf("xg%d" % i, [128, 128], F32) for i in range(2)]
            xn_tm = P.sbuf("xn_tm", [128, D], BF16)
            prod = [P.sbuf("prod0", [128, D], BF16)]
            rs_tm = P.sbuf("rs_tm", [128, 128], F32)
            accq = [P.sbuf("accq%d" % i, [128, 512], F32) for i in range(2)]
            NB = 5
            ubuf = [P.sbuf("ubuf%d" % i, [128, D], BF16) for i in range(3)] + [wvbA, wvbB]
            junk2 = P.sbuf("junk2", [128, D], BF16)
            convtok = P.sbuf("convtok", [128, 2], F32)
            convtokv = P.sbuf("convtokv", [128, 2], F32)
            identb = P.sbuf("identb", [128, 128], BF16)
            dg = [P.sbuf("dg%d" % i, [128, 128], BF16) for i in range(4)]
        psb = [P.psum("ps%d" % i, [128, 512], F32) for i in range(8)]
        rot = {"i": 0}

        def ps_next():
            t = psb[4 + rot["i"] % 4]
            rot["i"] += 1
            return t

        rot4 = {"i": 0}

        def ps_next4():
            t = psb[4 + rot4["i"] % 4]
            rot4["i"] += 1
            return t

        ident = con[:, 0, :]
        rmat = con[:, 1, :]

        def dbg_out(name, tile, ap, shape, dt=F32):
            if dbg is None or name not in dbg:
                return
            d = nc.dram_tensor("dbg_" + name, list(shape), dt, kind="ExternalOutput").ap()
            P.dma("sp", lambda e: e.dma_start(out=d, in_=ap), tile, False)
            dbg_outs[name] = tile

        wrot = {"i": 0}

        def wgroup(name, j):
            if name == "win":
                return 0 if j < CGA else 1
            if name in ("wao", "wco"):
                return 1
            return 2

        def load_w(name, j, kc=DC):
            t = wt[wrot["i"] % NW]
            wrot["i"] += 1
            src_ap = wbf[name][j]
            tok = convw[wgroup(name, j)]
            P.dma("sp", lambda e: e.dma_start(out=t[:, 0:kc, :], in_=src_ap), t, True, extra_reads=[tok])
            return t

        def proj(ps, n, wtile, rhs_tile, rhs_fn, kc=DC):
            for k in range(kc):
                P.op("pe", lambda e, k=k: e.matmul(ps[:, 0:n], lhsT=wtile[:, k, :], rhs=rhs_fn(k),
                                                    start=(k == 0), stop=(k == kc - 1)),
                     [wtile, rhs_tile], [ps])

        def norm(x_tile, n, gbase, out_tile, rstd):
            ps = ps_next()
            for dc in range(DC):
                sq = nsq[dc % 2]
                P.op("act", lambda e, dc=dc, sq=sq: e.activation(out=sq[:, 0:n], in_=x_tile[:, dc, 0:n], func=AF.Square),
                     [x_tile], [sq])
                P.op("pe", lambda e, dc=dc, sq=sq: e.matmul(ps[:, 0:n], lhsT=ones_bf[:], rhs=sq[:, 0:n],
                                                            start=(dc == 0), stop=(dc == DC - 1)),
                     [ones_bf, sq], [ps])
            P.op("act", lambda e: e.activation(out=nln[:, 0:n], in_=ps[:, 0:n], func=AF.Ln, scale=1.0 / D, bias=EPS),
                 [ps], [nln])
            P.op("act", lambda e: e.activation(out=rstd[:, 0:n], in_=nln[:, 0:n], func=AF.Exp, scale=-0.5),
                 [nln], [rstd])
            for dc in range(DC):
                P.op("dve", lambda e, dc=dc: e.scalar_tensor_tensor(
                    out=out_tile[:, dc, 0:n], in0=x_tile[:, dc, 0:n], scalar=vec[:, gbase + dc:gbase + dc + 1],
                    in1=rstd[:, 0:n], op0=ALU.mult, op1=ALU.mult), [x_tile, vec, rstd], [out_tile])

        def qkrope(ps, gcol, out_tile, out_ap, cst):
            n = T
            P.op("act", lambda e: e.activation(out=qsq[:, 0:n], in_=ps[:, 0:n], func=AF.Square), [ps], [qsq])
            ps2 = ps_next()
            P.op("pe", lambda e: e.matmul(ps2[:, 0:n], lhsT=ones_bf[:], rhs=qsq[:, 0:n], start=True, stop=True),
                 [ones_bf, qsq], [ps2])
            P.op("act", lambda e: e.activation(out=qln[:, 0:n], in_=ps2[:, 0:n], func=AF.Ln, scale=1.0 / HD, bias=EPS),
                 [ps2], [qln])
            P.op("act", lambda e: e.activation(out=qrs[:, 0:n], in_=qln[:, 0:n], func=AF.Exp, scale=-0.5), [qln], [qrs])
            P.op("dve", lambda e: e.scalar_tensor_tensor(out=qn[:, 0:n], in0=ps[:, 0:n], scalar=vec[:, gcol:gcol + 1],
                                                         in1=qrs[:, 0:n], op0=ALU.mult, op1=ALU.mult),
                 [ps, vec, qrs], [qn])
            ps3 = ps_next()
            P.op("pe", lambda e: e.matmul(ps3[:, 0:n], lhsT=rmat, rhs=qn[:, 0:n], start=True, stop=True),
                 [con, qn], [ps3])
            P.op("dve", lambda e: e.tensor_tensor(out=qt1[:, 0:n], in0=qn[:, 0:n], in1=cst[:, 0, :], op=ALU.mult),
                 [qn, cst], [qt1])
            P.op("dve", lambda e: e.tensor_tensor(out=qt2[:, 0:n], in0=ps3[:, 0:n], in1=cst[:, 1, :], op=ALU.mult),
                 [ps3, cst], [qt2])
            P.op("dve", lambda e: e.tensor_tensor(out=out_ap, in0=qt1[:, 0:n], in1=qt2[:, 0:n], op=ALU.add),
                 [qt1, qt2], [out_tile])

        P.dma("sp", lambda e: e.dma_start(out=vec[:], in_=vecs[:, :]), vec, True)
        P.dma("sp", lambda e: e.dma_start(out=con[:], in_=consts[:, :, :]), con, True)
        P.dma("sp", lambda e: e.dma_start(out=gb[:, 0, :], in_=gqk[0:1, :].partition_broadcast(128)), gb, True)
        P.dma("sp", lambda e: e.dma_start(out=gb[:, 1, :], in_=gqk[1:2, :].partition_broadcast(128)), gb, True)
        P.op("dve", lambda e: e.memset(ones_bf[:], 1.0), [], [ones_bf])
        P.op("dve", lambda e: e.tensor_reduce(out=gmax[:], in_=gb[:], axis=AX.X, op=ALU.max, apply_absolute_value=True),
             [gb], [gmax])
        P.op("dve", lambda e: e.tensor_scalar(out=negc[:], in0=gmax[:, 0:1], scalar1=gmax[:, 1:2], scalar2=-float(np.sqrt(HD)),
                                              op0=ALU.mult, op1=ALU.mult), [gmax], [negc])
        P.dma("pool", lambda e: e.dma_start(out=wvb3[0], in_=wv[:, 0:8, :]), wvbA, True)
        P.dma("pool", lambda e: e.dma_start(out=wvb3[1], in_=wv[:, 8:16, :]), wvbB, True)
        for kvh in range(2):
            P.dma("pool", lambda e, kvh=kvh: e.dma_start(out=wk[kvh][:], in_=win[CK + kvh]), wk[kvh], True)
        if do_peer:
            P.dma("sp", lambda e: e.dma_start(out=kT[:], in_=keysT[:, :, :]), kT, True)

        jobs = []
        order = [("win", j) for j in range(0, 8)] + [("win", j) for j in range(CCH, CGA)]
        order += [("wao", j) for j in range(16)] + [("wco", j) for j in range(16)] + [("win", j) for j in range(CGA, 68)]
        for nm in ("wo", "wpq", "wpg", "wpl"):
            order += [(nm, j) for j in range(16)]
        for (name, j) in order:
            a, n, kc = wsrc[name]
            jobs.append((a[j].rearrange("p k c -> p (k c)"), wbf[name][j].rearrange("p k c -> p (k c)"), kc * 128,
                         convw[wgroup(name, j)]))
        if do_peer:
            P.op("dve", lambda e: e.tensor_copy(out=identb[:], in_=ident), [con], [identb])
            for (src, dst, tk) in ((pu, pu_bf, convtok), (pv, pv_bf, convtokv)):
                for r in range(NEXP // 128):
                    jobs.append((src[r * 128:(r + 1) * 128, :], dst[r * 128:(r + 1) * 128, :], D, tk))
            stg = [ubuf[0], ubuf[1], ubuf[2], xn_tm, junk2]
        else:
            stg = [P.sbuf("stg%d" % i, [128, D], BF16) for i in range(4)]
        LAG = len(stg) // 2

        def cv_load(i):
            src, dst, w, tok = jobs[i]
            st = stg[i % len(stg)]
            P.dma("pool", lambda e: e.dma_start(out=st[:, 0:w], in_=src), st, True)

        def cv_store(i):
            src, dst, w, tok = jobs[i]
            st = stg[i % len(stg)]
            P.dma("pool", lambda e: e.dma_start(out=dst, in_=st[:, 0:w]), tok, True, extra_reads=[st])

        for i in range(len(jobs) + LAG):
            if i < len(jobs):
                cv_load(i)
            if i - LAG >= 0:
                cv_store(i - LAG)

        for ta in range(NTA):
            c0 = ta * T
            P.dma("sp", lambda e, c0=c0: e.dma_start(out=xa[:], in_=xT_v[:, :, c0:c0 + T]), xa, True)
            P.dma("sp", lambda e, c0=c0: e.dma_start(out=cst[:], in_=cs[:, :, c0:c0 + T]), cst, True)
            norm(xa, T, VG_MIX, hT, rstd)
            for kvh in range(2):
                w = wk[kvh]
                ps = ps_next()
                proj(ps, T, w, hT, lambda k: hT[:, k, :])
                qkrope(ps, VG_K, KT, KT[:, kvh, c0:c0 + T], cst)
            for sub in range(T // 128):
                ps = ps_next()
                for k in range(DC):
                    P.op("pe", lambda e, k=k, sub=sub, ps=ps: e.matmul(
                        ps[:, 0:256], lhsT=hT[:, k, sub * 128:(sub + 1) * 128], rhs=wvb3[k // 8][:, k % 8, :],
                        start=(k == 0), stop=(k == DC - 1)), [hT, wvbA, wvbB], [ps])
                vi = ta * (T // 128) + sub
                P.op("act", lambda e, ps=ps, vi=vi: e.activation(out=V[:, vi, :], in_=ps[:, 0:256], func=AF.Copy),
                     [ps], [V])
        dbg_out("KT", KT, KT[:, :, 0:512], [128, 2, 512], BF16)
        dbg_out("V", V, V[:, 0:4, :], [128, 4, 256], BF16)

        psO = psb[0]
        psL = psb[1]
        SCALE = float(HD) ** -0.5

        def bind(par):
            return xaL[par], hTL[par], xhL[par], hhL[par], cstL[par], rstdL[par]

        def mixer_units(tb, par):
            xa, hT, xh, hh, cst, rstd = bind(par)
            units = []

            def unit():

                c0 = tb * T
                P.dma("sp", lambda e, c0=c0: e.dma_start(out=xa[:], in_=xT_v[:, :, c0:c0 + T]), xa, True)
                P.dma("sp", lambda e, c0=c0: e.dma_start(out=cst[:], in_=cs[:, :, c0:c0 + T]), cst, True)
                P.dma("sp", lambda e, tb=tb: e.dma_start(out=xh[:], in_=xhalo[:, tb, :, :]), xh, True)
                norm(xh, 2, VG_MIX, hh, rstd)
                norm(xa, T, VG_MIX, hT, rstd)

            units.append((unit, False, False))

            for h in range(8):
                def unit(h=h):
                    w = load_w("win", CQ + h)
                    ps = ps_next()
                    proj(ps, T, w, hT, lambda k: hT[:, k, :])
                    qkrope(ps, VG_Q, qT, qT3[:, h, :], cst)

                units.append((unit, False, False))

            for h in range(8):
                kvh = h // 4
                NSC = SEQ // 128
                NSP = NSC // 2
                state = {}

                def s_mm(sp, kvh=kvh, h=h):
                    pS = ps_next()
                    for j in range(2):
                        sc = 2 * sp + j
                        P.op("pe", lambda e, pS=pS, sc=sc, j=j: e.matmul(
                            pS[:, j * T:(j + 1) * T], lhsT=KT[:, kvh, sc * 128:(sc + 1) * 128], rhs=qT3[:, h, :],
                            start=True, stop=True), [KT, qT], [pS])
                    return pS

                for part in range(4):
                    def unit(h=h, kvh=kvh, part=part, state=state, s_mm=s_mm, NSP=NSP, NSC=NSC):
                        if part == 0:
                            state["next"] = s_mm(0)
                        for sp in range(part * 4, part * 4 + 4):
                            pS = state["next"]
                            if sp + 1 < NSP:
                                state["next"] = s_mm(sp + 1)
                            px = pex[sp % 3]
                            P.op("act", lambda e, pS=pS, px=px: e.activation(out=px[:], in_=pS[:, 0:2 * T], func=AF.Exp,
                                                                              scale=SCALE, bias=negc[:, 0:1]),
                                 [pS, negc], [px])
                            for j in range(2):
                                sc = 2 * sp + j
                                P.op("pe", lambda e, px=px, sc=sc, kvh=kvh, j=j: e.matmul(
                                    psO[:, 0:T], lhsT=V[:, sc, kvh * 128:(kvh + 1) * 128], rhs=px[:, j * T:(j + 1) * T],
                                    start=(sc == 0), stop=(sc == NSC - 1)), [V, px], [psO])
                                P.op("pe", lambda e, px=px, sc=sc, j=j: e.matmul(
                                    psL[:, 0:T], lhsT=ones_bf[:], rhs=px[:, j * T:(j + 1) * T],
                                    start=(sc == 0), stop=(sc == NSC - 1)), [ones_bf, px], [psL])
                        if part == 3:
                            P.op("dve", lambda e: e.reciprocal(out=rl[:], in_=psL[:, 0:T]), [psL], [rl])
                            P.op("dve", lambda e, h=h: e.tensor_tensor(out=oT3[:, h, :], in0=psO[:, 0:T], in1=rl[:], op=ALU.mult),
                                 [psO, rl], [oT])

                    units.append((unit, True, part > 0))

            for ci in range(8):
                def unit(ci=ci):
                    w_h = load_w("win", CCH + ci)
                    pH = ps_next()
                    proj(pH, T, w_h, hT, lambda k: hT[:, k, :])
                    pHh = ps_next()
                    proj(pHh, 2, w_h, hh, lambda k: hh[:, k, :])
                    w_c = load_w("win", CCC + ci)
                    pC = ps_next()
                    proj(pC, T, w_c, hT, lambda k: hT[:, k, :])
                    pCh = ps_next()
                    proj(pCh, 2, w_c, hh, lambda k: hh[:, k, :])
                    P.op("act", lambda e, pH=pH: e.activation(out=ctmp[:, 1:T + 1], in_=pH[:, 0:T], func=AF.Copy), [pH], [ctmp])
                    P.op("act", lambda e, pHh=pHh: e.activation(out=ctmp[:, 0:1], in_=pHh[:, 0:1], func=AF.Copy), [pHh], [ctmp])
                    P.op("act", lambda e, pHh=pHh: e.activation(out=ctmp[:, T + 1:T + 2], in_=pHh[:, 1:2], func=AF.Copy), [pHh], [ctmp])
                    P.op("dve", lambda e, pC=pC: e.tensor_tensor(out=u[:, 1:T + 1], in0=ctmp[:, 1:T + 1], in1=pC[:, 0:T], op=ALU.mult),
                         [ctmp, pC], [u])
                    P.op("dve", lambda e, pCh=pCh: e.tensor_tensor(out=u[:, 0:1], in0=ctmp[:, 0:1], in1=pCh[:, 0:1], op=ALU.mult),
                         [ctmp, pCh], [u])
                    P.op("dve", lambda e, pCh=pCh: e.tensor_tensor(out=u[:, T + 1:T + 2], in0=ctmp[:, T + 1:T + 2], in1=pCh[:, 1:2], op=ALU.mult),
                         [ctmp, pCh], [u])
                    wc0 = VCW + ci * 3
                    P.op("dve", lambda e, wc0=wc0: e.tensor_scalar(out=cacc[:], in0=u[:, 0:T], scalar1=vec[:, wc0:wc0 + 1], scalar2=None,
                                                                    op0=ALU.mult), [u, vec], [cacc])
                    for kk in (1, 2):
                        P.op("dve", lambda e, wc0=wc0, kk=kk: e.scalar_tensor_tensor(
                            out=cacc[:], in0=u[:, kk:kk + T], scalar=vec[:, wc0 + kk:wc0 + kk + 1], in1=cacc[:],
                            op0=ALU.mult, op1=ALU.add), [u, vec, cacc], [cacc])
                    w_b = load_w("win", CCB + ci)
                    pB = ps_next()
                    proj(pB, T, w_b, hT, lambda k: hT[:, k, :])
                    P.op("dve", lambda e, pB=pB, ci=ci: e.tensor_tensor(out=cvT3[:, ci, :], in0=cacc[:], in1=pB[:, 0:T], op=ALU.mult),
                         [cacc, pB], [cvT])

                units.append((unit, False, False))

            for mc in range(DC):
                def unit(mc=mc):
                    w_a = load_w("wao", mc, kc=8)
                    pA = ps_next()
                    proj(pA, T, w_a, oT, lambda k: oT3[:, k, :], kc=8)
                    w_c = load_w("wco", mc, kc=8)
                    pC = ps_next()
                    proj(pC, T, w_c, cvT, lambda k: cvT3[:, k, :], kc=8)
                    w_ga = load_w("win", CGA + mc)
                    pGA = ps_next()
                    proj(pGA, T, w_ga, hT, lambda k: hT[:, k, :])
                    w_gc = load_w("win", CGC + mc)
                    pGC = ps_next()
                    proj(pGC, T, w_gc, hT, lambda k: hT[:, k, :])
                    P.op("act", lambda e, pGA=pGA: e.activation(out=sga[:], in_=pGA[:, 0:T], func=AF.Sigmoid), [pGA], [sga])
                    P.op("act", lambda e, pGC=pGC: e.activation(out=sgc[:], in_=pGC[:, 0:T], func=AF.Sigmoid), [pGC], [sgc])
                    P.op("dve", lambda e, pA=pA: e.tensor_tensor(out=m1[:], in0=sga[:], in1=pA[:, 0:T], op=ALU.mult), [sga, pA], [m1])
                    P.op("dve", lambda e, pC=pC: e.tensor_tensor(out=m2[:], in0=sgc[:], in1=pC[:, 0:T], op=ALU.mult), [sgc, pC], [m2])
                    P.op("dve", lambda e, mc=mc: e.tensor_tensor(out=mg[:, mc, :], in0=m1[:], in1=m2[:], op=ALU.add), [m1, m2], [mg])

                units.append((unit, False, False))

            for oc in range(DC):
                def unit(oc=oc):
                    w = load_w("wo", oc)
                    ps = ps_next()
                    proj(ps, T, w, mg, lambda k: mg[:, k, :])
                    P.op("dve", lambda e, ps=ps, oc=oc: e.tensor_tensor(out=xa[:, oc, :], in0=xa[:, oc, :], in1=ps[:, 0:T], op=ALU.add),
                         [xa, ps], [xa])

                units.append((unit, False, False))

            return units


        def pre_steps(par, sub):
            xa, hT, xh, hh, cst, rstd = bind(par)
            s0 = sub * 128
            sv, si, eidi = svL[sub], siL[sub], eidiL[sub]
            steps = []

            def st():
                pR = ps_next()
                P.op("pe", lambda e, pR=pR: e.transpose(pR[:, 0:128], rstd[:, s0:s0 + 128], ident), [rstd, con], [pR])
                P.op("act", lambda e, pR=pR: e.activation(out=rs_tm[:], in_=pR[:, 0:128], func=AF.Copy), [pR], [rs_tm])
            steps.append(st)
            for dc in range(DC):
                def st(dc=dc):
                    xgt = xg[dc % 2]
                    P.op("dve", lambda e, xgt=xgt: e.tensor_scalar(
                        out=xgt[:], in0=xa[:, dc, s0:s0 + 128], scalar1=vec[:, VG_FFN + dc:VG_FFN + dc + 1], scalar2=None,
                        op0=ALU.mult), [xa, vec], [xgt])
                    pX = ps_next()
                    P.op("pe", lambda e, pX=pX, xgt=xgt: e.transpose(pX[:, 0:128], xgt[:], ident), [xgt, con], [pX])
                    P.op("act", lambda e, pX=pX: e.activation(out=xn_tm[:, dc * 128:(dc + 1) * 128], in_=pX[:, 0:128], func=AF.Copy),
                         [pX], [xn_tm])
                steps.append(st)

            def st():
                P.op("dve", lambda e: e.tensor_copy(out=sif[:], in_=si[:]), [si], [sif])
            steps.append(st)
            for h in range(8):
                def st(h=h):
                    a0 = sv[:, 2 * h, :].unsqueeze(2).to_broadcast([128, 16, 16])
                    a1 = sv[:, 2 * h + 1, :].unsqueeze(1).to_broadcast([128, 16, 16])
                    i0 = sif[:, 2 * h, :].unsqueeze(2).to_broadcast([128, 16, 16])
                    i1 = sif[:, 2 * h + 1, :].unsqueeze(1).to_broadcast([128, 16, 16])
                    cv3 = cand[:].rearrange("p (a b) -> p a b", a=16)
                    ci3 = cidx[:].rearrange("p (a b) -> p a b", a=16)
                    P.op("dve", lambda e: e.tensor_tensor(out=cv3, in0=a0, in1=a1, op=ALU.add), [sv], [cand])
                    P.op("dve", lambda e: e.scalar_tensor_tensor(out=ci3, in0=i0, scalar=128.0, in1=i1,
                                                                 op0=ALU.mult, op1=ALU.add), [sif], [cidx])
                    P.op("dve", lambda e: e.max(out=tv[:, h, 0:8], in_=cand[:]), [cand], [tv])
                    P.op("dve", lambda e: e.match_replace(out=cand2[:], in_to_replace=tv[:, h, 0:8], in_values=cand[:], imm_value=-1e30),
                         [cand, tv], [cand2])
                    P.op("dve", lambda e: e.max(out=tv[:, h, 8:16], in_=cand2[:]), [cand2], [tv])
                steps.append(st)
                for kq in range(4):
                    def st(h=h, kq=kq):
                        for k in range(kq * 4, kq * 4 + 4):
                            P.op("dve", lambda e, k=k: e.scalar_tensor_tensor(
                                out=junk[:], in0=cand[:], scalar=tv[:, h, k:k + 1], in1=cidx[:], op0=ALU.is_equal, op1=ALU.mult,
                                accum_out=eidf[:, h * 16 + k:h * 16 + k + 1]), [cand, tv, cidx], [], nowait=[eidf])
                    steps.append(st)

            def st():
                P.op("dve", lambda e: e.tensor_scalar(out=eidi[:], in0=eidf[:], scalar1=float(NEXP - 1), scalar2=0.0,
                                                      op0=ALU.min, op1=ALU.max), [eidf], [eidi])
                P.op("dve", lambda e: e.tensor_scalar(out=negm[:], in0=tv[:, :, 0], scalar1=-1.0, scalar2=None, op0=ALU.mult), [tv], [negm])
                for h in range(8):
                    P.op("act", lambda e, h=h: e.activation(out=ge[:, h, :], in_=tv[:, h, :], func=AF.Exp, bias=negm[:, h:h + 1],
                                                            accum_out=gsum[:, h:h + 1]), [tv, negm], [ge, gsum])
                P.op("dve", lambda e: e.reciprocal(out=rgs[:], in_=gsum[:]), [gsum], [rgs])
                P.op("dve", lambda e: e.tensor_tensor(out=gate[:].rearrange("p (h k) -> p h k", h=8), in0=ge[:],
                                                      in1=rgs[:].unsqueeze(2).to_broadcast([128, 8, 16]), op=ALU.mult), [ge, rgs], [gate])
            steps.append(st)
            return steps

        def peer_front(par):
            xa, hT, xh, hh, cst, rstd = bind(par)
            steps = []

            def st():
                norm(xa, T, VG_FFN, hT, rstd)
            steps.append(st)
            for hp in range(16):
                def st(hp=hp):
                    w = load_w("wpq", hp)
                    pQ = ps_next()
                    proj(pQ, T, w, hT, lambda k: hT[:, k, :])
                    q_s = qs[hp % 2]
                    P.op("act", lambda e, pQ=pQ, q_s=q_s: e.activation(out=q_s[:], in_=pQ[:, 0:T], func=AF.Copy), [pQ], [q_s])
                    for sub in range(2):
                        sv, si = svL[sub], siL[sub]
                        pS = ps_next()
                        P.op("pe", lambda e, pS=pS, q_s=q_s, sub=sub: e.matmul(
                            pS[:, 0:128], lhsT=q_s[:, sub * 128:(sub + 1) * 128], rhs=kT[:, hp, :], start=True, stop=True),
                            [q_s, kT], [pS])
                        sc_t = sct[sub]
                        P.op("act", lambda e, pS=pS, sc_t=sc_t: e.activation(out=sc_t[:], in_=pS[:, 0:128], func=AF.Copy), [pS], [sc_t])
                        P.op("dve", lambda e, sc_t=sc_t, sv=sv: e.max(out=sv[:, hp, 0:8], in_=sc_t[:]), [sc_t], [sv])
                        P.op("dve", lambda e, sc_t=sc_t, sv=sv, si=si: e.max_index(out=si[:, hp, 0:8], in_max=sv[:, hp, 0:8], in_values=sc_t[:]),
                             [sc_t, sv], [si])
                        P.op("dve", lambda e, sc_t=sc_t, sv=sv: e.match_replace(out=sc2[:], in_to_replace=sv[:, hp, 0:8], in_values=sc_t[:],
                                                                                imm_value=-1e30), [sc_t, sv], [sc2])
                        P.op("dve", lambda e, sv=sv: e.max(out=sv[:, hp, 8:16], in_=sc2[:]), [sc2], [sv])
                        P.op("dve", lambda e, sv=sv, si=si: e.max_index(out=si[:, hp, 8:16], in_max=sv[:, hp, 8:16], in_values=sc2[:]),
                             [sc2, sv], [si])
                steps.append(st)
            return steps + pre_steps(par, 0)

        def peer_body(tb, par, pull, finish_open_head, drain, next_front):
            xa, hT, xh, hh, cst, rstd = bind(par)
            hidden = pre_steps(par, 1)
            for sub in range(T // 128):
                s0 = sub * 128
                eidi = eidiL[sub]
                if sub == 1:
                    for st in hidden:
                        st()
                    hidden = []
                P.op("dve", lambda e: e.memset(araw[:], 0.0), [], [araw])
                P.op("dve", lambda e: e.memset(araw2[:], 0.0), [], [araw2])
                def u_consume(hk):
                    ub = ubuf[hk % NB]
                    if (hk % 3 != 0) if sub == 0 else (hk % 2 == 0):
                        P.op("dve", lambda e, ub=ub, hk=hk: e.scalar_tensor_tensor(
                            out=junk2[:], in0=ub[:], scalar=1.0, in1=xn_tm[:], op0=ALU.mult, op1=ALU.mult,
                            accum_out=araw[:, hk:hk + 1]), [ub, xn_tm], [], nowait=[araw])
                    else:
                        pr = prod[0]
                        P.op("dve", lambda e, ub=ub, pr=pr: e.tensor_tensor(out=pr[:], in0=ub[:], in1=xn_tm[:], op=ALU.mult),
                             [ub, xn_tm], [pr])
                        P.op("act", lambda e, pr=pr, hk=hk: e.activation(out=pr[:], in_=pr[:], func=AF.Copy,
                                                                          accum_out=araw2[:, hk:hk + 1]), [pr], [pr], nowait=[araw2])

                for hk in range(128 + CLAG):
                    if hk < 128:
                        ub = ubuf[hk % NB]
                        P.dma("pool", lambda e, ub=ub, hk=hk, eidi=eidi: e.indirect_dma_start(
                            out=ub[:], out_offset=None, in_=pu_bf[:, :],
                            in_offset=bass.IndirectOffsetOnAxis(ap=eidi[:, hk:hk + 1], axis=0)), ub, True, extra_reads=[eidi, convtok])
                    if hk % PULL_EVERY == 0:
                        pull(True)
                    if hk - CLAG >= 0:
                        u_consume(hk - CLAG)
                finish_open_head()
                P.op("dve", lambda e: e.tensor_tensor(out=araw[:], in0=araw[:], in1=araw2[:], op=ALU.add), [araw, araw2], [araw])
                P.op("dve", lambda e: e.tensor_scalar(out=asc[:], in0=araw[:], scalar1=rs_tm[:, 0:1], scalar2=None, op0=ALU.mult),
                     [araw, rs_tm], [asc])
                P.op("act", lambda e: e.activation(out=hid[:], in_=asc[:], func=AF.Gelu), [asc], [hid])
                P.op("dve", lambda e: e.tensor_tensor(out=hid[:], in0=hid[:], in1=gate[:], op=ALU.mult), [hid, gate], [hid])
                if sub == 1:
                    drain()
                psV = [psb[0], psb[1], psb[2], psb[3]]
                def v_consume(hk):
                    ub = ubuf[hk % NB]
                    dgt = dg[hk % 4]
                    P.op("dve", lambda e, dgt=dgt, hk=hk: e.tensor_scalar(out=dgt[:], in0=identb[:], scalar1=hid[:, hk:hk + 1], scalar2=None,
                                                                          op0=ALU.mult), [identb, hid], [dgt])
                    for dq in range(4):
                        P.op("pe", lambda e, dgt=dgt, ub=ub, dq=dq, hk=hk: e.matmul(
                            psV[dq][:, 0:512], lhsT=dgt[:], rhs=ub[:, dq * 512:(dq + 1) * 512],
                            start=(hk == 0), stop=(hk == 127)), [dgt, ub], [psV[dq]])

                for hk in range(128 + CLAG):
                    if hk < 128:
                        ub = ubuf[hk % NB]
                        P.dma("pool", lambda e, ub=ub, hk=hk, eidi=eidi: e.indirect_dma_start(
                            out=ub[:], out_offset=None, in_=pv_bf[:, :],
                            in_offset=bass.IndirectOffsetOnAxis(ap=eidi[:, hk:hk + 1], axis=0)), ub, True, extra_reads=[eidi, convtokv])
                    if hk - CLAG >= 0:
                        v_consume(hk - CLAG)
                    if sub == 0 and hidden and hk % 2 == 1:
                        hidden.pop(0)()
                    if sub == 1 and next_front and hk >= 2:
                        next_front.pop(0)()
                if tb == 0 and sub == 0:
                    dbg_out("eidf", eidf, eidf[:], [128, 128], F32)
                    dbg_out("gate", gate, gate[:], [128, 128], F32)
                    dbg_out("asc", asc, asc[:], [128, 128], F32)
                for dq in range(4):
                    aq = accq[dq % 2]
                    P.op("act", lambda e, dq=dq, aq=aq: e.activation(out=aq[:], in_=psV[dq][:, 0:512], func=AF.Copy), [psV[dq]], [aq])
                    for j in range(4):
                        dc = dq * 4 + j
                        pX = ps_next4()
                        P.op("pe", lambda e, pX=pX, j=j, aq=aq: e.transpose(pX[:, 0:128], aq[:, j * 128:(j + 1) * 128], ident), [aq, con], [pX])
                        P.op("dve", lambda e, pX=pX, dc=dc, s0=s0: e.tensor_tensor(
                            out=xa[:, dc, s0:s0 + 128], in0=xa[:, dc, s0:s0 + 128], in1=pX[:, 0:128], op=ALU.add), [xa, pX], [xa])


        def ple_units(tb, par):
            xa, hT, xh, hh, cst, rstd = bind(par)
            c0 = tb * T
            units = []

            def unit():
                norm(xa, T, VG_PLE, hT, rstd)
                P.dma("pool", lambda e, c0=c0: e.dma_start(out=ptb[:], in_=pT_v[:, :, c0:c0 + T]), ptb, True)

            units.append((unit, False, False))
            for oc in range(DC):
                def unit(oc=oc):
                    w_g = load_w("wpg", oc)
                    pG = ps_next()
                    proj(pG, T, w_g, hT, lambda k: hT[:, k, :])
                    w_p = load_w("wpl", oc, kc=2)
                    pP = ps_next()
                    proj(pP, T, w_p, ptb, lambda k: ptb[:, k, :], kc=2)
                    P.op("act", lambda e, pG=pG: e.activation(out=sga[:], in_=pG[:, 0:T], func=AF.Sigmoid), [pG], [sga])
                    P.op("dve", lambda e, pP=pP: e.tensor_tensor(out=m1[:], in0=sga[:], in1=pP[:, 0:T], op=ALU.mult), [sga, pP], [m1])
                    P.op("dve", lambda e, oc=oc: e.tensor_tensor(out=xa[:, oc, :], in0=xa[:, oc, :], in1=m1[:], op=ALU.add), [xa, m1], [xa])
                    if oc == DC - 1:
                        P.dma("sp", lambda e, c0=c0: e.dma_start(out=outT_v[:, :, c0:c0 + T], in_=xa[:]), xa, False)

                units.append((unit, False, False))
            return units

        PULL_EVERY = 2
        CLAG = 3
        pending = {"units": [], "i": 0}

        def pull(allow01):
            u, i = pending["units"], pending["i"]
            if i < len(u) and (allow01 or not u[i][1]):
                pending["i"] = i + 1
                u[i][0]()

        def finish_open_head():
            u = pending["units"]
            while pending["i"] < len(u) and u[pending["i"]][2]:
                pull(True)

        def drain():
            while pending["i"] < len(pending["units"]):
                pull(True)

        for (fn, _a, _b) in mixer_units(0, 0):
            fn()
        carry = []
        front = peer_front(0) if do_peer else []
        for tb in range(nt_run):
            par = tb % 2
            pending["units"] = carry + (mixer_units(tb + 1, 1 - par) if tb + 1 < nt_run else [])
            pending["i"] = 0
            if do_peer:
                for st in front:
                    st()
                nxt = peer_front(1 - par) if tb + 1 < nt_run else []
                peer_body(tb, par, pull, finish_open_head, drain, nxt)
                front = nxt
            drain()
            if tb == 0:
                dbg_out("x2", xaL[0], xaL[0][:], [128, DC, T], F32)
            carry = ple_units(tb, par)
        for (fn, _a, _b) in carry:
            fn()

        P.final_wait("sp", xaL + list(dbg_outs.values()))
        P.emit()
        n_ops = P.n_ops
    return nc, n_ops


def _lay(W):
    K, N = W.shape
    return np.ascontiguousarray(W.reshape(K // 128, 128, N // 128, 128).transpose(2, 1, 0, 3))


def _rope_tables():
    S = SEQ
    rows = S // 64
    row = np.repeat(np.arange(rows, dtype=np.int32), 64)[:S]
    col = np.tile(np.arange(64, dtype=np.int32), rows)
    inv = (np.float32(10000.0) ** (-np.arange(0, 64, 2, dtype=np.float32) / np.float32(64))).astype(np.float32)
    ang_r = row.astype(np.float32)[:, None] * inv[None, :]
    ang_c = col.astype(np.float32)[:, None] * inv[None, :]
    ang = np.concatenate([ang_r, ang_r, ang_c, ang_c], axis=-1)
    return np.cos(ang).astype(np.float32), np.sin(ang).astype(np.float32)


def _consts():
    c = np.zeros((128, 2, 128), np.float32)
    c[:, 0, :] = np.eye(128, dtype=np.float32)
    for a in range(2):
        for f in range(32):
            i0 = a * 64 + f
            i1 = a * 64 + 32 + f
            c[i1, 1, i0] = -1.0
            c[i0, 1, i1] = 1.0
    return c


def prepare_inputs(x, p, g_mix, w_in, g_q, g_k, conv_w, w_attn_out, w_conv_out, w_out,
                   g_ffn, w_peer_q, peer_sub_keys, peer_u, peer_v, g_ple, w_ple, w_ple_gate):
    f = lambda a: np.asarray(a, dtype=np.float32)
    x = f(x); p = f(p)[0]
    w_in = f(w_in)[0]
    shared = {}
    shared["win"] = _lay(w_in)
    shared["wv"] = np.ascontiguousarray(w_in[:, 1280:1536].reshape(16, 128, 256).transpose(1, 0, 2))
    shared["wao"] = _lay(f(w_attn_out)[0])
    shared["wco"] = _lay(f(w_conv_out)[0])
    shared["wo"] = _lay(f(w_out)[0])
    shared["wpq"] = _lay(f(w_peer_q)[0])
    shared["wpg"] = _lay(f(w_ple_gate)[0])
    shared["wpl"] = _lay(f(w_ple)[0])
    sk = f(peer_sub_keys)[0].reshape(16, 128, 128)
    shared["keysT"] = np.ascontiguousarray(sk.transpose(2, 0, 1))
    shared["pu"] = np.ascontiguousarray(f(peer_u)[0])
    shared["pv"] = np.ascontiguousarray(f(peer_v)[0])
    vecs = np.zeros((128, NVEC), np.float32)
    vecs[:, VG_MIX:VG_MIX + 16] = f(g_mix)[0].reshape(16, 128).T
    vecs[:, VG_FFN:VG_FFN + 16] = f(g_ffn)[0].reshape(16, 128).T
    vecs[:, VG_PLE:VG_PLE + 16] = f(g_ple)[0].reshape(16, 128).T
    vecs[:, VG_Q] = f(g_q)[0]
    vecs[:, VG_K] = f(g_k)[0]
    cw = f(conv_w)[0]
    vecs[:, VCW:VCW + 24] = cw.reshape(3, 8, 128).transpose(2, 1, 0).reshape(128, 24)
    shared["vecs"] = vecs
    shared["gqk"] = np.stack([f(g_q)[0], f(g_k)[0]], axis=0)
    shared["consts"] = _consts()
    cos, sin = _rope_tables()
    in_maps = []
    for c in range(8):
        b, half = c // 2, c % 2
        own = slice(half * OWN, (half + 1) * OWN)
        oth = slice((1 - half) * OWN, (2 - half) * OWN)
        xb = x[b]
        m = dict(shared)
        m["xT"] = np.ascontiguousarray(np.concatenate([xb[own], xb[oth]], axis=0).T)
        xh = np.zeros((NT, 2, D), np.float32)
        for tb in range(NT):
            l = half * OWN + tb * T - 1
            r = half * OWN + (tb + 1) * T
            if l >= 0:
                xh[tb, 0] = xb[l]
            if r < SEQ:
                xh[tb, 1] = xb[r]
        m["xhalo"] = np.ascontiguousarray(xh.reshape(NT, 2, DC, 128).transpose(3, 0, 2, 1))
        m["pT"] = np.ascontiguousarray(p[b, own].T)
        cso = np.stack([np.concatenate([cos[own], cos[oth]], axis=0).T,
                        np.concatenate([sin[own], sin[oth]], axis=0).T], axis=1)
        m["cs"] = np.ascontiguousarray(cso)
        in_maps.append(m)
    return in_maps


_CACHE = {}


def kernel(**inputs):
    in_maps = prepare_inputs(**inputs)
    if "nc" not in _CACHE:
        _CACHE["nc"] = build_program()[0]
    nc = _CACHE["nc"]
    res = run_bass_kernel_spmd(nc, in_maps, core_ids=list(range(8)))
    out = np.empty((4, SEQ, D), np.float32)
    for c in range(8):
        b, half = c // 2, c % 2
        out[b, half * OWN:(half + 1) * OWN, :] = np.asarray(res.results[c]["outT"]).T
    return out
```

```python
import os
import numpy as np
from contextlib import ExitStack
import concourse.bass as bass
import concourse.mybir as mybir
from concourse.bass_utils import run_bass_kernel_spmd

F32 = mybir.dt.float32
BF16 = mybir.dt.bfloat16
U32 = mybir.dt.uint32
I32 = mybir.dt.int32
AF = mybir.ActivationFunctionType
ALU = mybir.AluOpType
AX = mybir.AxisListType

ENGS = ["pe", "act", "dve", "pool", "sp"]
NOSELF = set(os.environ.get("K_NOSELF", "").split(",")) - {""}


class Tile:
    def __init__(self, prog, handle, name):
        self.prog = prog
        self.h = handle
        self.name = name
        self.wev = None
        self.revs = []
        self.dsem = None
        self.dcnt = 0

    def __getitem__(self, idx):
        return self.h[idx]


class Prog:
    def __init__(self, nc, es):
        self.nc = nc
        self.es = es
        self.q = {e: [] for e in ENGS}
        self.cnt = {e: 0 for e in ENGS}
        self.sem = {}
        for e in ["pe", "act", "dve", "pool"]:
            self.sem[e] = es.enter_context(nc.semaphore("s_" + e))
        self.seen = {e: {} for e in ENGS}
        self.n_ops = 0

    def sbuf(self, name, shape, dt):
        h = self.es.enter_context(self.nc.sbuf_tensor(name, list(shape), dt))
        return Tile(self, h, name)

    def psum(self, name, shape, dt):
        h = self.es.enter_context(self.nc.psum_tensor(name, list(shape), dt))
        return Tile(self, h, name)

    def _dsem(self, t):
        if t.dsem is None:
            t.dsem = self.es.enter_context(self.nc.semaphore("d_" + t.name))
        return t.dsem

    def _collect(self, eng, reads, writes, is_dma):
        waits = []
        for t in reads:
            if t.wev is not None:
                waits.append(t.wev)
        for t in writes:
            if t.wev is not None:
                if not (is_dma and t.wev[3]):
                    waits.append(t.wev)
            waits.extend(t.revs)
        out = []
        seen = self.seen[eng]
        for (sem, val, weng, wdma) in waits:
            if eng == "pe" and weng == "pe" and not wdma:
                continue
            if weng == eng and not wdma and eng in NOSELF:
                continue
            key = id(sem)
            if seen.get(key, 0) >= val:
                continue
            seen[key] = val
            out.append((sem, val))
        return out

    def op(self, eng, fn, reads=(), writes=(), nowait=()):
        reads = [t for t in reads if t is not None]
        writes = [t for t in writes if t is not None]
        waits = self._collect(eng, reads, writes, False)
        writes = writes + list(nowait)
        self.cnt[eng] += 1
        ev = (self.sem[eng], self.cnt[eng], eng, False)
        self.q[eng].append((waits, fn, (self.sem[eng], 1)))
        for t in writes:
            t.wev = ev
            t.revs = []
        for t in reads:
            if t not in writes:
                t.revs = [r for r in t.revs if r[0] is not ev[0]] + [ev]
        self.n_ops += 1

    def dma(self, eng, fn, tile, is_write, extra_reads=()):
        reads = list(extra_reads) + ([] if is_write else [tile])
        writes = [tile] if is_write else []
        waits = self._collect(eng, reads, writes, True)
        sem = self._dsem(tile)
        tile.dcnt += 16
        ev = (sem, tile.dcnt, eng, True)
        self.q[eng].append((waits, fn, (sem, 16)))
        if is_write:
            tile.wev = ev
            tile.revs = []
        else:
            tile.revs = [r for r in tile.revs if r[0] is not sem] + [ev]
        for t in extra_reads:
            t.revs = [r for r in t.revs if r[0] is not sem] + [ev]
        self.n_ops += 1

    def final_wait(self, eng, tiles):
        waits = []
        for t in tiles:
            waits.extend(t.revs)
            if t.wev is not None:
                waits.append(t.wev)
        best = {}
        for (sem, val, _e, _d) in waits:
            k = id(sem)
            if k not in best or best[k][1] < val:
                best[k] = (sem, val)
        self.q[eng].append((list(best.values()), None, None))

    def emit(self):
        nc = self.nc

        def replay(ename, e):
            for (waits, fn, inc) in self.q[ename]:
                for (sem, val) in waits:
                    e.wait_ge(sem, val)
                if fn is not None:
                    ins = fn(e)
                    ins.then_inc(inc[0], inc[1])

        with nc.Block() as block:
            @block.tensor
            def _(e):
                replay("pe", e)

            @block.scalar
            def _(e):
                replay("act", e)

            @block.vector
            def _(e):
                replay("dve", e)

            @block.gpsimd
            def _(e):
                replay("pool", e)

            @block.sync
            def _(e):
                replay("sp", e)


D = 2048
DC = 16
SEQ = 4096
OWN = 2048
T = 256
NT = OWN // T
NTA = SEQ // T
HD = 128
NEXP = 16384
EPS = 1e-6
CQ, CK, CCH, CCB, CCC, CGA, CGC = 0, 8, 12, 20, 28, 36, 52
VG_MIX, VG_FFN, VG_PLE, VG_Q, VG_K, VCW = 0, 16, 32, 48, 49, 50
NVEC = 80


def build_program(nt_run=NT, do_peer=True, dbg=None):
    nc = bass.Bass("TRN2", target_bir_lowering=False)

    def din(name, shape, dt=F32):
        return nc.dram_tensor(name, list(shape), dt, kind="ExternalInput").ap()

    xT = din("xT", [D, SEQ])
    xhalo = din("xhalo", [128, NT, DC, 2])
    pT = din("pT", [256, OWN])
    cs = din("cs", [128, 2, SEQ])
    vecs = din("vecs", [128, NVEC])
    gqk = din("gqk", [2, 128])
    consts = din("consts", [128, 2, 128])
    win = din("win", [68, 128, DC, 128])
    wv = din("wv", [128, DC, 256])
    wao = din("wao", [16, 128, 8, 128])
    wco = din("wco", [16, 128, 8, 128])
    wo = din("wo", [16, 128, DC, 128])
    wpq = din("wpq", [16, 128, DC, 128])
    wpg = din("wpg", [16, 128, DC, 128])
    wpl = din("wpl", [16, 128, 2, 128])
    keysT = din("keysT", [128, 16, 128])
    pu = din("pu", [NEXP, D])
    pv = din("pv", [NEXP, D])
    outT = nc.dram_tensor("outT", [D, OWN], F32, kind="ExternalOutput").ap()
    pu_bf = nc.dram_tensor("pu_bf", [NEXP, D], BF16, kind="Internal").ap()
    pv_bf = nc.dram_tensor("pv_bf", [NEXP, D], BF16, kind="Internal").ap()
    wsrc = {"win": (win, 68, DC), "wao": (wao, 16, 8), "wco": (wco, 16, 8), "wo": (wo, 16, DC),
            "wpq": (wpq, 16, DC), "wpg": (wpg, 16, DC), "wpl": (wpl, 16, 2)}
    wbf = {k: nc.dram_tensor(k + "_bf", [n, 128, kc, 128], BF16, kind="Internal").ap() for k, (a, n, kc) in wsrc.items()}
    dbg_outs = {}

    xT_v = xT.rearrange("(dc p) t -> p dc t", p=128)
    outT_v = outT.rearrange("(dc p) t -> p dc t", p=128)
    pT_v = pT.rearrange("(kc p) t -> p kc t", p=128)

    with ExitStack() as es:
        P = Prog(nc, es)
        xaL = [P.sbuf("xa%d" % i, [128, DC, T], F32) for i in range(2)]
        hTL = [P.sbuf("hT%d" % i, [128, DC, T], BF16) for i in range(2)]
        xhL = [P.sbuf("xh%d" % i, [128, DC, 2], F32) for i in range(2)]
        hhL = [P.sbuf("hh%d" % i, [128, DC, 2], BF16) for i in range(2)]
        nsq = [P.sbuf("nsq%d" % i, [128, T], BF16) for i in range(2)]
        nln = P.sbuf("nln", [128, T], F32)
        rstdL = [P.sbuf("rstd%d" % i, [128, T], F32) for i in range(2)]
        KT = P.sbuf("KT", [128, 2, SEQ], BF16)
        V = P.sbuf("V", [128, SEQ // 128, 256], BF16)
        cstL = [P.sbuf("cst%d" % i, [128, 2, T], F32) for i in range(2)]
        xa, hT, xh, hh, cst, rstd = xaL[0], hTL[0], xhL[0], hhL[0], cstL[0], rstdL[0]
        vec = P.sbuf("vec", [128, NVEC], F32)
        con = P.sbuf("con", [128, 2, 128], F32)
        ones_bf = P.sbuf("ones_bf", [128, 128], BF16)
        gb = P.sbuf("gb", [128, 2, 128], F32)
        gmax = P.sbuf("gmax", [128, 2], F32)
        negc = P.sbuf("negc", [128, 1], F32)
        NW = 3
        wt = [P.sbuf("wt%d" % i, [128, DC, 128], BF16) for i in range(NW)]
        wvbA = P.sbuf("wvbA", [128, 8 * 256], BF16)
        wvbB = P.sbuf("wvbB", [128, 8 * 256], BF16)
        wvb3 = [wvbA[:].rearrange("p (k c) -> p k c", k=8), wvbB[:].rearrange("p (k c) -> p k c", k=8)]
        wk = [wt[0], wt[1]]
        convw = [P.sbuf("convw%d" % i, [128, 2], F32) for i in range(3)]
        qT = P.sbuf("qT", [128, 8 * T], BF16)
        oT = P.sbuf("oT", [128, 8 * T], BF16)
        cvT = P.sbuf("cvT", [128, 8 * T], BF16)
        qT3 = qT[:].rearrange("p (h t) -> p h t", h=8)
        oT3 = oT[:].rearrange("p (h t) -> p h t", h=8)
        cvT3 = cvT[:].rearrange("p (h t) -> p h t", h=8)
        mg = P.sbuf("mg", [128, DC, T], BF16)
        pex = [P.sbuf("pex%d" % i, [128, 2 * T], BF16) for i in range(3)]
        qsq = P.sbuf("qsq", [128, T], BF16)
        qln = P.sbuf("qln", [128, T], F32)
        qrs = P.sbuf("qrs", [128, T], F32)
        qn = P.sbuf("qn", [128, T], F32)
        qt1 = P.sbuf("qt1", [128, T], F32)
        qt2 = P.sbuf("qt2", [128, T], F32)
        rl = P.sbuf("rl", [128, T], F32)
        u = P.sbuf("u", [128, T + 2], F32)
        ctmp = P.sbuf("ctmp", [128, T + 2], F32)
        cacc = P.sbuf("cacc", [128, T], F32)
        sga = P.sbuf("sga", [128, T], F32)
        sgc = P.sbuf("sgc", [128, T], F32)
        m1 = P.sbuf("m1", [128, T], F32)
        m2 = P.sbuf("m2", [128, T], F32)
        ptb = P.sbuf("ptb", [128, 2, T], BF16)
        if do_peer:
            kT = P.sbuf("kTs", [128, 16, 128], F32)
            qs = [P.sbuf("qs%d" % i, [128, T], F32) for i in range(2)]
            sct = [P.sbuf("sct%d" % i, [128, 128], F32) for i in range(2)]
            sc2 = P.sbuf("sc2", [128, 128], F32)
            svL = [P.sbuf("sv%d" % i, [128, 16, 16], F32) for i in range(2)]
            siL = [P.sbuf("si%d" % i, [128, 16, 16], U32) for i in range(2)]
            sif = P.sbuf("sif", [128, 16, 16], F32)
            cand = P.sbuf("cand", [128, 256], F32)
            cand2 = P.sbuf("cand2", [128, 256], F32)
            cidx = P.sbuf("cidx", [128, 256], F32)
            junk = P.sbuf("junk", [128, 256], F32)
            tv = P.sbuf("tv", [128, 8, 16], F32)
            eidf = P.sbuf("eidf", [128, 128], F32)
            eidiL = [P.sbuf("eidi%d" % i, [128, 128], U32) for i in range(2)]
            negm = P.sbuf("negm", [128, 8], F32)
            ge = P.sbuf("ge", [128, 8, 16], F32)
            gsum = P.sbuf("gsum", [128, 8], F32)
            rgs = P.sbuf("rgs", [128, 8], F32)
            gate = P.sbuf("gate", [128, 128], F32)
            araw = P.sbuf("araw", [128, 128], F32)
            araw2 = P.sbuf("araw2", [128, 128], F32)
            asc = P.sbuf("asc", [128, 128], F32)
            hid = P.sbuf("hid", [128, 128], F32)
            xg = [P.sbuf("xg%d" % i, [128, 128], F32) for i in range(2)]
            xn_tm = P.sbuf("xn_tm", [128, D], BF16)
            prod = [P.sbuf("prod0", [128, D], BF16)]
            rs_tm = P.sbuf("rs_tm", [128, 128], F32)
            accq = [P.sbuf("accq%d" % i, [128, 512], F32) for i in range(2)]
            NB = 5
            ubuf = [P.sbuf("ubuf%d" % i, [128, D], BF16) for i in range(3)] + [wvbA, wvbB]
            junk2 = P.sbuf("junk2", [128, D], BF16)
            convtok = P.sbuf("convtok", [128, 2], F32)
            convtokv = P.sbuf("convtokv", [128, 2], F32)
            identb = P.sbuf("identb", [128, 128], BF16)
            dg = [P.sbuf("dg%d" % i, [128, 128], BF16) for i in range(4)]
        psb = [P.psum("ps%d" % i, [128, 512], F32) for i in range(8)]
        rot = {"i": 0}

        def ps_next():
            t = psb[4 + rot["i"] % 4]
            rot["i"] += 1
            return t

        rot4 = {"i": 0}

        def ps_next4():
            t = psb[4 + rot4["i"] % 4]
            rot4["i"] += 1
            return t

        ident = con[:, 0, :]
        rmat = con[:, 1, :]

        def dbg_out(name, tile, ap, shape, dt=F32):
            if dbg is None or name not in dbg:
                return
            d = nc.dram_tensor("dbg_" + name, list(shape), dt, kind="ExternalOutput").ap()
            P.dma("sp", lambda e: e.dma_start(out=d, in_=ap), tile, False)
            dbg_outs[name] = tile

        wrot = {"i": 0}

        def wgroup(name, j):
            if name == "win":
                return 0 if j < CGA else 1
            if name in ("wao", "wco"):
                return 1
            return 2

        def load_w(name, j, kc=DC):
            t = wt[wrot["i"] % NW]
            wrot["i"] += 1
            src_ap = wbf[name][j]
            tok = convw[wgroup(name, j)]
            P.dma("sp", lambda e: e.dma_start(out=t[:, 0:kc, :], in_=src_ap), t, True, extra_reads=[tok])
            return t

        def proj(ps, n, wtile, rhs_tile, rhs_fn, kc=DC):
            for k in range(kc):
                P.op("pe", lambda e, k=k: e.matmul(ps[:, 0:n], lhsT=wtile[:, k, :], rhs=rhs_fn(k),
                                                    start=(k == 0), stop=(k == kc - 1)),
                     [wtile, rhs_tile], [ps])

        def norm(x_tile, n, gbase, out_tile, rstd):
            ps = ps_next()
            for dc in range(DC):
                sq = nsq[dc % 2]
                P.op("act", lambda e, dc=dc, sq=sq: e.activation(out=sq[:, 0:n], in_=x_tile[:, dc, 0:n], func=AF.Square),
                     [x_tile], [sq])
                P.op("pe", lambda e, dc=dc, sq=sq: e.matmul(ps[:, 0:n], lhsT=ones_bf[:], rhs=sq[:, 0:n],
                                                            start=(dc == 0), stop=(dc == DC - 1)),
                     [ones_bf, sq], [ps])
            P.op("act", lambda e: e.activation(out=nln[:, 0:n], in_=ps[:, 0:n], func=AF.Ln, scale=1.0 / D, bias=EPS),
                 [ps], [nln])
            P.op("act", lambda e: e.activation(out=rstd[:, 0:n], in_=nln[:, 0:n], func=AF.Exp, scale=-0.5),
                 [nln], [rstd])
            for dc in range(DC):
                P.op("dve", lambda e, dc=dc: e.scalar_tensor_tensor(
                    out=out_tile[:, dc, 0:n], in0=x_tile[:, dc, 0:n], scalar=vec[:, gbase + dc:gbase + dc + 1],
                    in1=rstd[:, 0:n], op0=ALU.mult, op1=ALU.mult), [x_tile, vec, rstd], [out_tile])

        def qkrope(ps, gcol, out_tile, out_ap, cst):
            n = T
            P.op("act", lambda e: e.activation(out=qsq[:, 0:n], in_=ps[:, 0:n], func=AF.Square), [ps], [qsq])
            ps2 = ps_next()
            P.op("pe", lambda e: e.matmul(ps2[:, 0:n], lhsT=ones_bf[:], rhs=qsq[:, 0:n], start=True, stop=True),
                 [ones_bf, qsq], [ps2])
            P.op("act", lambda e: e.activation(out=qln[:, 0:n], in_=ps2[:, 0:n], func=AF.Ln, scale=1.0 / HD, bias=EPS),
                 [ps2], [qln])
            P.op("act", lambda e: e.activation(out=qrs[:, 0:n], in_=qln[:, 0:n], func=AF.Exp, scale=-0.5), [qln], [qrs])
            P.op("dve", lambda e: e.scalar_tensor_tensor(out=qn[:, 0:n], in0=ps[:, 0:n], scalar=vec[:, gcol:gcol + 1],
                                                         in1=qrs[:, 0:n], op0=ALU.mult, op1=ALU.mult),
                 [ps, vec, qrs], [qn])
            ps3 = ps_next()
            P.op("pe", lambda e: e.matmul(ps3[:, 0:n], lhsT=rmat, rhs=qn[:, 0:n], start=True, stop=True),
                 [con, qn], [ps3])
            P.op("dve", lambda e: e.tensor_tensor(out=qt1[:, 0:n], in0=qn[:, 0:n], in1=cst[:, 0, :], op=ALU.mult),
                 [qn, cst], [qt1])
            P.op("dve", lambda e: e.tensor_tensor(out=qt2[:, 0:n], in0=ps3[:, 0:n], in1=cst[:, 1, :], op=ALU.mult),
                 [ps3, cst], [qt2])
            P.op("dve", lambda e: e.tensor_tensor(out=out_ap, in0=qt1[:, 0:n], in1=qt2[:, 0:n], op=ALU.add),
                 [qt1, qt2], [out_tile])

        P.dma("sp", lambda e: e.dma_start(out=vec[:], in_=vecs[:, :]), vec, True)
        P.dma("sp", lambda e: e.dma_start(out=con[:], in_=consts[:, :, :]), con, True)
        P.dma("sp", lambda e: e.dma_start(out=gb[:, 0, :], in_=gqk[0:1, :].partition_broadcast(128)), gb, True)
        P.dma("sp", lambda e: e.dma_start(out=gb[:, 1, :], in_=gqk[1:2, :].partition_broadcast(128)), gb, True)
        P.op("dve", lambda e: e.memset(ones_bf[:], 1.0), [], [ones_bf])
        P.op("dve", lambda e: e.tensor_reduce(out=gmax[:], in_=gb[:], axis=AX.X, op=ALU.max, apply_absolute_value=True),
             [gb], [gmax])
        P.op("dve", lambda e: e.tensor_scalar(out=negc[:], in0=gmax[:, 0:1], scalar1=gmax[:, 1:2], scalar2=-float(np.sqrt(HD)),
                                              op0=ALU.mult, op1=ALU.mult), [gmax], [negc])
        P.dma("pool", lambda e: e.dma_start(out=wvb3[0], in_=wv[:, 0:8, :]), wvbA, True)
        P.dma("pool", lambda e: e.dma_start(out=wvb3[1], in_=wv[:, 8:16, :]), wvbB, True)
        for kvh in range(2):
            P.dma("pool", lambda e, kvh=kvh: e.dma_start(out=wk[kvh][:], in_=win[CK + kvh]), wk[kvh], True)
        if do_peer:
            P.dma("sp", lambda e: e.dma_start(out=kT[:], in_=keysT[:, :, :]), kT, True)

        jobs = []
        order = [("win", j) for j in range(0, 8)] + [("win", j) for j in range(CCH, CGA)]
        order += [("wao", j) for j in range(16)] + [("wco", j) for j in range(16)] + [("win", j) for j in range(CGA, 68)]
        for nm in ("wo", "wpq", "wpg", "wpl"):
            order += [(nm, j) for j in range(16)]
        for (name, j) in order:
            a, n, kc = wsrc[name]
            jobs.append((a[j].rearrange("p k c -> p (k c)"), wbf[name][j].rearrange("p k c -> p (k c)"), kc * 128,
                         convw[wgroup(name, j)]))
        if do_peer:
            P.op("dve", lambda e: e.tensor_copy(out=identb[:], in_=ident), [con], [identb])
            for (src, dst, tk) in ((pu, pu_bf, convtok), (pv, pv_bf, convtokv)):
                for r in range(NEXP // 128):
                    jobs.append((src[r * 128:(r + 1) * 128, :], dst[r * 128:(r + 1) * 128, :], D, tk))
            stg = [ubuf[0], ubuf[1], ubuf[2], xn_tm, junk2]
        else:
            stg = [P.sbuf("stg%d" % i, [128, D], BF16) for i in range(4)]
        LAG = len(stg) // 2

        def cv_load(i):
            src, dst, w, tok = jobs[i]
            st = stg[i % len(stg)]
            P.dma("pool", lambda e: e.dma_start(out=st[:, 0:w], in_=src), st, True)

        def cv_store(i):
            src, dst, w, tok = jobs[i]
            st = stg[i % len(stg)]
            P.dma("pool", lambda e: e.dma_start(out=dst, in_=st[:, 0:w]), tok, True, extra_reads=[st])

        for i in range(len(jobs) + LAG):
            if i < len(jobs):
                cv_load(i)
            if i - LAG >= 0:
                cv_store(i - LAG)

        for ta in range(NTA):
            c0 = ta * T
            P.dma("sp", lambda e, c0=c0: e.dma_start(out=xa[:], in_=xT_v[:, :, c0:c0 + T]), xa, True)
            P.dma("sp", lambda e, c0=c0: e.dma_start(out=cst[:], in_=cs[:, :, c0:c0 + T]), cst, True)
            norm(xa, T, VG_MIX, hT, rstd)
            for kvh in range(2):
                w = wk[kvh]
                ps = ps_next()
                proj(ps, T, w, hT, lambda k: hT[:, k, :])
                qkrope(ps, VG_K, KT, KT[:, kvh, c0:c0 + T], cst)
            for sub in range(T // 128):
                ps = ps_next()
                for k in range(DC):
                    P.op("pe", lambda e, k=k, sub=sub, ps=ps: e.matmul(
                        ps[:, 0:256], lhsT=hT[:, k, sub * 128:(sub + 1) * 128], rhs=wvb3[k // 8][:, k % 8, :],
                        start=(k == 0), stop=(k == DC - 1)), [hT, wvbA, wvbB], [ps])
                vi = ta * (T // 128) + sub
                P.op("act", lambda e, ps=ps, vi=vi: e.activation(out=V[:, vi, :], in_=ps[:, 0:256], func=AF.Copy),
                     [ps], [V])
        dbg_out("KT", KT, KT[:, :, 0:512], [128, 2, 512], BF16)
        dbg_out("V", V, V[:, 0:4, :], [128, 4, 256], BF16)

        psO = psb[0]
        psL = psb[1]
        SCALE = float(HD) ** -0.5

        def bind(par):
            return xaL[par], hTL[par], xhL[par], hhL[par], cstL[par], rstdL[par]

        def mixer_units(tb, par):
            xa, hT, xh, hh, cst, rstd = bind(par)
            units = []

            def unit():

                c0 = tb * T
                P.dma("sp", lambda e, c0=c0: e.dma_start(out=xa[:], in_=xT_v[:, :, c0:c0 + T]), xa, True)
                P.dma("sp", lambda e, c0=c0: e.dma_start(out=cst[:], in_=cs[:, :, c0:c0 + T]), cst, True)
                P.dma("sp", lambda e, tb=tb: e.dma_start(out=xh[:], in_=xhalo[:, tb, :, :]), xh, True)
                norm(xh, 2, VG_MIX, hh, rstd)
                norm(xa, T, VG_MIX, hT, rstd)

            units.append((unit, False, False))

            for h in range(8):
                def unit(h=h):
                    w = load_w("win", CQ + h)
                    ps = ps_next()
                    proj(ps, T, w, hT, lambda k: hT[:, k, :])
                    qkrope(ps, VG_Q, qT, qT3[:, h, :], cst)

                units.append((unit, False, False))

            for h in range(8):
                kvh = h // 4
                NSC = SEQ // 128
                NSP = NSC // 2
                state = {}

                def s_mm(sp, kvh=kvh, h=h):
                    pS = ps_next()
                    for j in range(2):
                        sc = 2 * sp + j
                        P.op("pe", lambda e, pS=pS, sc=sc, j=j: e.matmul(
                            pS[:, j * T:(j + 1) * T], lhsT=KT[:, kvh, sc * 128:(sc + 1) * 128], rhs=qT3[:, h, :],
                            start=True, stop=True), [KT, qT], [pS])
                    return pS

                for part in range(4):
                    def unit(h=h, kvh=kvh, part=part, state=state, s_mm=s_mm, NSP=NSP, NSC=NSC):
                        if part == 0:
                            state["next"] = s_mm(0)
                        for sp in range(part * 4, part * 4 + 4):
                            pS = state["next"]
                            if sp + 1 < NSP:
                                state["next"] = s_mm(sp + 1)
                            px = pex[sp % 3]
                            P.op("act", lambda e, pS=pS, px=px: e.activation(out=px[:], in_=pS[:, 0:2 * T], func=AF.Exp,
                                                                              scale=SCALE, bias=negc[:, 0:1]),
                                 [pS, negc], [px])
                            for j in range(2):
                                sc = 2 * sp + j
                                P.op("pe", lambda e, px=px, sc=sc, kvh=kvh, j=j: e.matmul(
                                    psO[:, 0:T], lhsT=V[:, sc, kvh * 128:(kvh + 1) * 128], rhs=px[:, j * T:(j + 1) * T],
                                    start=(sc == 0), stop=(sc == NSC - 1)), [V, px], [psO])
                                P.op("pe", lambda e, px=px, sc=sc, j=j: e.matmul(
                                    psL[:, 0:T], lhsT=ones_bf[:], rhs=px[:, j * T:(j + 1) * T],
                                    start=(sc == 0), stop=(sc == NSC - 1)), [ones_bf, px], [psL])
                        if part == 3:
                            P.op("dve", lambda e: e.reciprocal(out=rl[:], in_=psL[:, 0:T]), [psL], [rl])
                            P.op("dve", lambda e, h=h: e.tensor_tensor(out=oT3[:, h, :], in0=psO[:, 0:T], in1=rl[:], op=ALU.mult),
                                 [psO, rl], [oT])

                    units.append((unit, True, part > 0))

            for ci in range(8):
                def unit(ci=ci):
                    w_h = load_w("win", CCH + ci)
                    pH = ps_next()
                    proj(pH, T, w_h, hT, lambda k: hT[:, k, :])
                    pHh = ps_next()
                    proj(pHh, 2, w_h, hh, lambda k: hh[:, k, :])
                    w_c = load_w("win", CCC + ci)
                    pC = ps_next()
                    proj(pC, T, w_c, hT, lambda k: hT[:, k, :])
                    pCh = ps_next()
                    proj(pCh, 2, w_c, hh, lambda k: hh[:, k, :])
                    P.op("act", lambda e, pH=pH: e.activation(out=ctmp[:, 1:T + 1], in_=pH[:, 0:T], func=AF.Copy), [pH], [ctmp])
                    P.op("act", lambda e, pHh=pHh: e.activation(out=ctmp[:, 0:1], in_=pHh[:, 0:1], func=AF.Copy), [pHh], [ctmp])
                    P.op("act", lambda e, pHh=pHh: e.activation(out=ctmp[:, T + 1:T + 2], in_=pHh[:, 1:2], func=AF.Copy), [pHh], [ctmp])
                    P.op("dve", lambda e, pC=pC: e.tensor_tensor(out=u[:, 1:T + 1], in0=ctmp[:, 1:T + 1], in1=pC[:, 0:T], op=ALU.mult),
                         [ctmp, pC], [u])
                    P.op("dve", lambda e, pCh=pCh: e.tensor_tensor(out=u[:, 0:1], in0=ctmp[:, 0:1], in1=pCh[:, 0:1], op=ALU.mult),
                         [ctmp, pCh], [u])
                    P.op("dve", lambda e, pCh=pCh: e.tensor_tensor(out=u[:, T + 1:T + 2], in0=ctmp[:, T + 1:T + 2], in1=pCh[:, 1:2], op=ALU.mult),
                         [ctmp, pCh], [u])
                    wc0 = VCW + ci * 3
                    P.op("dve", lambda e, wc0=wc0: e.tensor_scalar(out=cacc[:], in0=u[:, 0:T], scalar1=vec[:, wc0:wc0 + 1], scalar2=None,
                                                                    op0=ALU.mult), [u, vec], [cacc])
                    for kk in (1, 2):
                        P.op("dve", lambda e, wc0=wc0, kk=kk: e.scalar_tensor_tensor(
                            out=cacc[:], in0=u[:, kk:kk + T], scalar=vec[:, wc0 + kk:wc0 + kk + 1], in1=cacc[:],
                            op0=ALU.mult, op1=ALU.add), [u, vec, cacc], [cacc])
                    w_b = load_w("win", CCB + ci)
                    pB = ps_next()
                    proj(pB, T, w_b, hT, lambda k: hT[:, k, :])
                    P.op("dve", lambda e, pB=pB, ci=ci: e.tensor_tensor(out=cvT3[:, ci, :], in0=cacc[:], in1=pB[:, 0:T], op=ALU.mult),
                         [cacc, pB], [cvT])

                units.append((unit, False, False))

            for mc in range(DC):
                def unit(mc=mc):
                    w_a = load_w("wao", mc, kc=8)
                    pA = ps_next()
                    proj(pA, T, w_a, oT, lambda k: oT3[:, k, :], kc=8)
                    w_c = load_w("wco", mc, kc=8)
                    pC = ps_next()
                    proj(pC, T, w_c, cvT, lambda k: cvT3[:, k, :], kc=8)
                    w_ga = load_w("win", CGA + mc)
                    pGA = ps_next()
                    proj(pGA, T, w_ga, hT, lambda k: hT[:, k, :])
                    w_gc = load_w("win", CGC + mc)
                    pGC = ps_next()
                    proj(pGC, T, w_gc, hT, lambda k: hT[:, k, :])
                    P.op("act", lambda e, pGA=pGA: e.activation(out=sga[:], in_=pGA[:, 0:T], func=AF.Sigmoid), [pGA], [sga])
                    P.op("act", lambda e, pGC=pGC: e.activation(out=sgc[:], in_=pGC[:, 0:T], func=AF.Sigmoid), [pGC], [sgc])
                    P.op("dve", lambda e, pA=pA: e.tensor_tensor(out=m1[:], in0=sga[:], in1=pA[:, 0:T], op=ALU.mult), [sga, pA], [m1])
                    P.op("dve", lambda e, pC=pC: e.tensor_tensor(out=m2[:], in0=sgc[:], in1=pC[:, 0:T], op=ALU.mult), [sgc, pC], [m2])
                    P.op("dve", lambda e, mc=mc: e.tensor_tensor(out=mg[:, mc, :], in0=m1[:], in1=m2[:], op=ALU.add), [m1, m2], [mg])

                units.append((unit, False, False))

            for oc in range(DC):
                def unit(oc=oc):
                    w = load_w("wo", oc)
                    ps = ps_next()
                    proj(ps, T, w, mg, lambda k: mg[:, k, :])
                    P.op("dve", lambda e, ps=ps, oc=oc: e.tensor_tensor(out=xa[:, oc, :], in0=xa[:, oc, :], in1=ps[:, 0:T], op=ALU.add),
                         [xa, ps], [xa])

                units.append((unit, False, False))

            return units


        def pre_steps(par, sub):
            xa, hT, xh, hh, cst, rstd = bind(par)
            s0 = sub * 128
            sv, si, eidi = svL[sub], siL[sub], eidiL[sub]
            steps = []

            def st():
                pR = ps_next()
                P.op("pe", lambda e, pR=pR: e.transpose(pR[:, 0:128], rstd[:, s0:s0 + 128], ident), [rstd, con], [pR])
                P.op("act", lambda e, pR=pR: e.activation(out=rs_tm[:], in_=pR[:, 0:128], func=AF.Copy), [pR], [rs_tm])
            steps.append(st)
            for dc in range(DC):
                def st(dc=dc):
                    xgt = xg[dc % 2]
                    P.op("dve", lambda e, xgt=xgt: e.tensor_scalar(
                        out=xgt[:], in0=xa[:, dc, s0:s0 + 128], scalar1=vec[:, VG_FFN + dc:VG_FFN + dc + 1], scalar2=None,
                        op0=ALU.mult), [xa, vec], [xgt])
                    pX = ps_next()
                    P.op("pe", lambda e, pX=pX, xgt=xgt: e.transpose(pX[:, 0:128], xgt[:], ident), [xgt, con], [pX])
                    P.op("act", lambda e, pX=pX: e.activation(out=xn_tm[:, dc * 128:(dc + 1) * 128], in_=pX[:, 0:128], func=AF.Copy),
                         [pX], [xn_tm])
                steps.append(st)

            def st():
                P.op("dve", lambda e: e.tensor_copy(out=sif[:], in_=si[:]), [si], [sif])
            steps.append(st)
            for h in range(8):
                def st(h=h):
                    a0 = sv[:, 2 * h, :].unsqueeze(2).to_broadcast([128, 16, 16])
                    a1 = sv[:, 2 * h + 1, :].unsqueeze(1).to_broadcast([128, 16, 16])
                    i0 = sif[:, 2 * h, :].unsqueeze(2).to_broadcast([128, 16, 16])
                    i1 = sif[:, 2 * h + 1, :].unsqueeze(1).to_broadcast([128, 16, 16])
                    cv3 = cand[:].rearrange("p (a b) -> p a b", a=16)
                    ci3 = cidx[:].rearrange("p (a b) -> p a b", a=16)
                    P.op("dve", lambda e: e.tensor_tensor(out=cv3, in0=a0, in1=a1, op=ALU.add), [sv], [cand])
                    P.op("dve", lambda e: e.scalar_tensor_tensor(out=ci3, in0=i0, scalar=128.0, in1=i1,
                                                                 op0=ALU.mult, op1=ALU.add), [sif], [cidx])
                    P.op("dve", lambda e: e.max(out=tv[:, h, 0:8], in_=cand[:]), [cand], [tv])
                    P.op("dve", lambda e: e.match_replace(out=cand2[:], in_to_replace=tv[:, h, 0:8], in_values=cand[:], imm_value=-1e30),
                         [cand, tv], [cand2])
                    P.op("dve", lambda e: e.max(out=tv[:, h, 8:16], in_=cand2[:]), [cand2], [tv])
                steps.append(st)
                for kq in range(4):
                    def st(h=h, kq=kq):
                        for k in range(kq * 4, kq * 4 + 4):
                            P.op("dve", lambda e, k=k: e.scalar_tensor_tensor(
                                out=junk[:], in0=cand[:], scalar=tv[:, h, k:k + 1], in1=cidx[:], op0=ALU.is_equal, op1=ALU.mult,
                                accum_out=eidf[:, h * 16 + k:h * 16 + k + 1]), [cand, tv, cidx], [], nowait=[eidf])
                    steps.append(st)

            def st():
                P.op("dve", lambda e: e.tensor_scalar(out=eidi[:], in0=eidf[:], scalar1=float(NEXP - 1), scalar2=0.0,
                                                      op0=ALU.min, op1=ALU.max), [eidf], [eidi])
                P.op("dve", lambda e: e.tensor_scalar(out=negm[:], in0=tv[:, :, 0], scalar1=-1.0, scalar2=None, op0=ALU.mult), [tv], [negm])
                for h in range(8):
                    P.op("act", lambda e, h=h: e.activation(out=ge[:, h, :], in_=tv[:, h, :], func=AF.Exp, bias=negm[:, h:h + 1],
                                                            accum_out=gsum[:, h:h + 1]), [tv, negm], [ge, gsum])
                P.op("dve", lambda e: e.reciprocal(out=rgs[:], in_=gsum[:]), [gsum], [rgs])
                P.op("dve", lambda e: e.tensor_tensor(out=gate[:].rearrange("p (h k) -> p h k", h=8), in0=ge[:],
                                                      in1=rgs[:].unsqueeze(2).to_broadcast([128, 8, 16]), op=ALU.mult), [ge, rgs], [gate])
            steps.append(st)
            return steps

        def peer_front(par):
            xa, hT, xh, hh, cst, rstd = bind(par)
            steps = []

            def st():
                norm(xa, T, VG_FFN, hT, rstd)
            steps.append(st)
            for hp in range(16):
                def st(hp=hp):
                    w = load_w("wpq", hp)
                    pQ = ps_next()
                    proj(pQ, T, w, hT, lambda k: hT[:, k, :])
                    q_s = qs[hp % 2]
                    P.op("act", lambda e, pQ=pQ, q_s=q_s: e.activation(out=q_s[:], in_=pQ[:, 0:T], func=AF.Copy), [pQ], [q_s])
                    for sub in range(2):
                        sv, si = svL[sub], siL[sub]
                        pS = ps_next()
                        P.op("pe", lambda e, pS=pS, q_s=q_s, sub=sub: e.matmul(
                            pS[:, 0:128], lhsT=q_s[:, sub * 128:(sub + 1) * 128], rhs=kT[:, hp, :], start=True, stop=True),
                            [q_s, kT], [pS])
                        sc_t = sct[sub]
                        P.op("act", lambda e, pS=pS, sc_t=sc_t: e.activation(out=sc_t[:], in_=pS[:, 0:128], func=AF.Copy), [pS], [sc_t])
                        P.op("dve", lambda e, sc_t=sc_t, sv=sv: e.max(out=sv[:, hp, 0:8], in_=sc_t[:]), [sc_t], [sv])
                        P.op("dve", lambda e, sc_t=sc_t, sv=sv, si=si: e.max_index(out=si[:, hp, 0:8], in_max=sv[:, hp, 0:8], in_values=sc_t[:]),
                             [sc_t, sv], [si])
                        P.op("dve", lambda e, sc_t=sc_t, sv=sv: e.match_replace(out=sc2[:], in_to_replace=sv[:, hp, 0:8], in_values=sc_t[:],
                                                                                imm_value=-1e30), [sc_t, sv], [sc2])
                        P.op("dve", lambda e, sv=sv: e.max(out=sv[:, hp, 8:16], in_=sc2[:]), [sc2], [sv])
                        P.op("dve", lambda e, sv=sv, si=si: e.max_index(out=si[:, hp, 8:16], in_max=sv[:, hp, 8:16], in_values=sc2[:]),
                             [sc2, sv], [si])
                steps.append(st)
            return steps + pre_steps(par, 0)

        def peer_body(tb, par, pull, finish_open_head, drain, next_front):
            xa, hT, xh, hh, cst, rstd = bind(par)
            hidden = pre_steps(par, 1)
            for sub in range(T // 128):
                s0 = sub * 128
                eidi = eidiL[sub]
                if sub == 1:
                    for st in hidden:
                        st()
                    hidden = []
                P.op("dve", lambda e: e.memset(araw[:], 0.0), [], [araw])
                P.op("dve", lambda e: e.memset(araw2[:], 0.0), [], [araw2])
                def u_consume(hk):
                    ub = ubuf[hk % NB]
                    if (hk % 3 != 0) if sub == 0 else (hk % 2 == 0):
                        P.op("dve", lambda e, ub=ub, hk=hk: e.scalar_tensor_tensor(
                            out=junk2[:], in0=ub[:], scalar=1.0, in1=xn_tm[:], op0=ALU.mult, op1=ALU.mult,
                            accum_out=araw[:, hk:hk + 1]), [ub, xn_tm], [], nowait=[araw])
                    else:
                        pr = prod[0]
                        P.op("dve", lambda e, ub=ub, pr=pr: e.tensor_tensor(out=pr[:], in0=ub[:], in1=xn_tm[:], op=ALU.mult),
                             [ub, xn_tm], [pr])
                        P.op("act", lambda e, pr=pr, hk=hk: e.activation(out=pr[:], in_=pr[:], func=AF.Copy,
                                                                          accum_out=araw2[:, hk:hk + 1]), [pr], [pr], nowait=[araw2])

                for hk in range(128 + CLAG):
                    if hk < 128:
                        ub = ubuf[hk % NB]
                        P.dma("pool", lambda e, ub=ub, hk=hk, eidi=eidi: e.indirect_dma_start(
                            out=ub[:], out_offset=None, in_=pu_bf[:, :],
                            in_offset=bass.IndirectOffsetOnAxis(ap=eidi[:, hk:hk + 1], axis=0)), ub, True, extra_reads=[eidi, convtok])
                    if hk % (3 if sub == 0 else 2) == 0:
                        pull(True)
                    if hk - CLAG >= 0:
                        u_consume(hk - CLAG)
                finish_open_head()
                P.op("dve", lambda e: e.tensor_tensor(out=araw[:], in0=araw[:], in1=araw2[:], op=ALU.add), [araw, araw2], [araw])
                P.op("dve", lambda e: e.tensor_scalar(out=asc[:], in0=araw[:], scalar1=rs_tm[:, 0:1], scalar2=None, op0=ALU.mult),
                     [araw, rs_tm], [asc])
                P.op("act", lambda e: e.activation(out=hid[:], in_=asc[:], func=AF.Gelu), [asc], [hid])
                P.op("dve", lambda e: e.tensor_tensor(out=hid[:], in0=hid[:], in1=gate[:], op=ALU.mult), [hid, gate], [hid])
                if sub == 1:
                    drain()
                psV = [psb[0], psb[1], psb[2], psb[3]]
                def v_consume(hk):
                    ub = ubuf[hk % NB]
                    dgt = dg[hk % 4]
                    P.op("dve", lambda e, dgt=dgt, hk=hk: e.tensor_scalar(out=dgt[:], in0=identb[:], scalar1=hid[:, hk:hk + 1], scalar2=None,
                                                                          op0=ALU.mult), [identb, hid], [dgt])
                    for dq in range(4):
                        P.op("pe", lambda e, dgt=dgt, ub=ub, dq=dq, hk=hk: e.matmul(
                            psV[dq][:, 0:512], lhsT=dgt[:], rhs=ub[:, dq * 512:(dq + 1) * 512],
                            start=(hk == 0), stop=(hk == 127)), [dgt, ub], [psV[dq]])

                for hk in range(128 + CLAG):
                    if hk < 128:
                        ub = ubuf[hk % NB]
                        P.dma("pool", lambda e, ub=ub, hk=hk, eidi=eidi: e.indirect_dma_start(
                            out=ub[:], out_offset=None, in_=pv_bf[:, :],
                            in_offset=bass.IndirectOffsetOnAxis(ap=eidi[:, hk:hk + 1], axis=0)), ub, True, extra_reads=[eidi, convtokv])
                    if hk - CLAG >= 0:
                        v_consume(hk - CLAG)
                    if sub == 0 and hidden and hk % 2 == 1:
                        hidden.pop(0)()
                    if sub == 1 and next_front and hk >= 2:
                        next_front.pop(0)()
                if tb == 0 and sub == 0:
                    dbg_out("eidf", eidf, eidf[:], [128, 128], F32)
                    dbg_out("gate", gate, gate[:], [128, 128], F32)
                    dbg_out("asc", asc, asc[:], [128, 128], F32)
                for dq in range(4):
                    aq = accq[dq % 2]
                    P.op("act", lambda e, dq=dq, aq=aq: e.activation(out=aq[:], in_=psV[dq][:, 0:512], func=AF.Copy), [psV[dq]], [aq])
                    for j in range(4):
                        dc = dq * 4 + j
                        pX = ps_next4()
                        P.op("pe", lambda e, pX=pX, j=j, aq=aq: e.transpose(pX[:, 0:128], aq[:, j * 128:(j + 1) * 128], ident), [aq, con], [pX])
                        P.op("dve", lambda e, pX=pX, dc=dc, s0=s0: e.tensor_tensor(
                            out=xa[:, dc, s0:s0 + 128], in0=xa[:, dc, s0:s0 + 128], in1=pX[:, 0:128], op=ALU.add), [xa, pX], [xa])


        def ple_units(tb, par):
            xa, hT, xh, hh, cst, rstd = bind(par)
            c0 = tb * T
            units = []

            def unit():
                norm(xa, T, VG_PLE, hT, rstd)
                P.dma("pool", lambda e, c0=c0: e.dma_start(out=ptb[:], in_=pT_v[:, :, c0:c0 + T]), ptb, True)

            units.append((unit, False, False))
            for oc in range(DC):
                def unit(oc=oc):
                    w_g = load_w("wpg", oc)
                    pG = ps_next()
                    proj(pG, T, w_g, hT, lambda k: hT[:, k, :])
                    w_p = load_w("wpl", oc, kc=2)
                    pP = ps_next()
                    proj(pP, T, w_p, ptb, lambda k: ptb[:, k, :], kc=2)
                    P.op("act", lambda e, pG=pG: e.activation(out=sga[:], in_=pG[:, 0:T], func=AF.Sigmoid), [pG], [sga])
                    P.op("dve", lambda e, pP=pP: e.tensor_tensor(out=m1[:], in0=sga[:], in1=pP[:, 0:T], op=ALU.mult), [sga, pP], [m1])
                    P.op("dve", lambda e, oc=oc: e.tensor_tensor(out=xa[:, oc, :], in0=xa[:, oc, :], in1=m1[:], op=ALU.add), [xa, m1], [xa])
                    if oc == DC - 1:
                        P.dma("sp", lambda e, c0=c0: e.dma_start(out=outT_v[:, :, c0:c0 + T], in_=xa[:]), xa, False)

                units.append((unit, False, False))
            return units

        PULL_EVERY = 2
        CLAG = 3
        pending = {"units": [], "i": 0}

        def pull(allow01):
            u, i = pending["units"], pending["i"]
            if i < len(u) and (allow01 or not u[i][1]):
                pending["i"] = i + 1
                u[i][0]()

        def finish_open_head():
            u = pending["units"]
            while pending["i"] < len(u) and u[pending["i"]][2]:
                pull(True)

        def drain():
            while pending["i"] < len(pending["units"]):
                pull(True)

        for (fn, _a, _b) in mixer_units(0, 0):
            fn()
        carry = []
        front = peer_front(0) if do_peer else []
        for tb in range(nt_run):
            par = tb % 2
            pending["units"] = carry + (mixer_units(tb + 1, 1 - par) if tb + 1 < nt_run else [])
            pending["i"] = 0
            if do_peer:
                for st in front:
                    st()
                nxt = peer_front(1 - par) if tb + 1 < nt_run else []
                peer_body(tb, par, pull, finish_open_head, drain, nxt)
                front = nxt
            drain()
            if tb == 0:
                dbg_out("x2", xaL[0], xaL[0][:], [128, DC, T], F32)
            carry = ple_units(tb, par)
        for (fn, _a, _b) in carry:
            fn()

        P.final_wait("sp", xaL + list(dbg_outs.values()))
        P.emit()
        n_ops = P.n_ops
    return nc, n_ops


def _lay(W):
    K, N = W.shape
    return np.ascontiguousarray(W.reshape(K // 128, 128, N // 128, 128).transpose(2, 1, 0, 3))


def _rope_tables():
    S = SEQ
    rows = S // 64
    row = np.repeat(np.arange(rows, dtype=np.int32), 64)[:S]
    col = np.tile(np.arange(64, dtype=np.int32), rows)
    inv = (np.float32(10000.0) ** (-np.arange(0, 64, 2, dtype=np.float32) / np.float32(64))).astype(np.float32)
    ang_r = row.astype(np.float32)[:, None] * inv[None, :]
    ang_c = col.astype(np.float32)[:, None] * inv[None, :]
    ang = np.concatenate([ang_r, ang_r, ang_c, ang_c], axis=-1)
    return np.cos(ang).astype(np.float32), np.sin(ang).astype(np.float32)


def _consts():
    c = np.zeros((128, 2, 128), np.float32)
    c[:, 0, :] = np.eye(128, dtype=np.float32)
    for a in range(2):
        for f in range(32):
            i0 = a * 64 + f
            i1 = a * 64 + 32 + f
            c[i1, 1, i0] = -1.0
            c[i0, 1, i1] = 1.0
    return c


def prepare_inputs(x, p, g_mix, w_in, g_q, g_k, conv_w, w_attn_out, w_conv_out, w_out,
                   g_ffn, w_peer_q, peer_sub_keys, peer_u, peer_v, g_ple, w_ple, w_ple_gate):
    f = lambda a: np.asarray(a, dtype=np.float32)
    x = f(x); p = f(p)[0]
    w_in = f(w_in)[0]
    shared = {}
    shared["win"] = _lay(w_in)
    shared["wv"] = np.ascontiguousarray(w_in[:, 1280:1536].reshape(16, 128, 256).transpose(1, 0, 2))
    shared["wao"] = _lay(f(w_attn_out)[0])
    shared["wco"] = _lay(f(w_conv_out)[0])
    shared["wo"] = _lay(f(w_out)[0])
    shared["wpq"] = _lay(f(w_peer_q)[0])
    shared["wpg"] = _lay(f(w_ple_gate)[0])
    shared["wpl"] = _lay(f(w_ple)[0])
    sk = f(peer_sub_keys)[0].reshape(16, 128, 128)
    shared["keysT"] = np.ascontiguousarray(sk.transpose(2, 0, 1))
    shared["pu"] = np.ascontiguousarray(f(peer_u)[0])
    shared["pv"] = np.ascontiguousarray(f(peer_v)[0])
    vecs = np.zeros((128, NVEC), np.float32)
    vecs[:, VG_MIX:VG_MIX + 16] = f(g_mix)[0].reshape(16, 128).T
    vecs[:, VG_FFN:VG_FFN + 16] = f(g_ffn)[0].reshape(16, 128).T
    vecs[:, VG_PLE:VG_PLE + 16] = f(g_ple)[0].reshape(16, 128).T
    vecs[:, VG_Q] = f(g_q)[0]
    vecs[:, VG_K] = f(g_k)[0]
    cw = f(conv_w)[0]
    vecs[:, VCW:VCW + 24] = cw.reshape(3, 8, 128).transpose(2, 1, 0).reshape(128, 24)
    shared["vecs"] = vecs
    shared["gqk"] = np.stack([f(g_q)[0], f(g_k)[0]], axis=0)
    shared["consts"] = _consts()
    cos, sin = _rope_tables()
    in_maps = []
    for c in range(8):
        b, half = c // 2, c % 2
        own = slice(half * OWN, (half + 1) * OWN)
        oth = slice((1 - half) * OWN, (2 - half) * OWN)
        xb = x[b]
        m = dict(shared)
        m["xT"] = np.ascontiguousarray(np.concatenate([xb[own], xb[oth]], axis=0).T)
        xh = np.zeros((NT, 2, D), np.float32)
        for tb in range(NT):
            l = half * OWN + tb * T - 1
            r = half * OWN + (tb + 1) * T
            if l >= 0:
                xh[tb, 0] = xb[l]
            if r < SEQ:
                xh[tb, 1] = xb[r]
        m["xhalo"] = np.ascontiguousarray(xh.reshape(NT, 2, DC, 128).transpose(3, 0, 2, 1))
        m["pT"] = np.ascontiguousarray(p[b, own].T)
        cso = np.stack([np.concatenate([cos[own], cos[oth]], axis=0).T,
                        np.concatenate([sin[own], sin[oth]], axis=0).T], axis=1)
        m["cs"] = np.ascontiguousarray(cso)
        in_maps.append(m)
    return in_maps


_CACHE = {}


def kernel(**inputs):
    in_maps = prepare_inputs(**inputs)
    if "nc" not in _CACHE:
        _CACHE["nc"] = build_program()[0]
    nc = _CACHE["nc"]
    res = run_bass_kernel_spmd(nc, in_maps, core_ids=list(range(8)))
    out = np.empty((4, SEQ, D), np.float32)
    for c in range(8):
        b, half = c // 2, c % 2
        out[b, half * OWN:(half + 1) * OWN, :] = np.asarray(res.results[c]["outT"]).T
    return out
```
